# Optimizing a Trainium2 kernel written in Bass

```python
import jax, jax.numpy as jnp
from jax import lax
import numpy as np

D_MODEL = 2048
BATCH = 16
SEQ = 2048
DEPTH = 1

D_MIX = D_MODEL
HEAD_CH = 64
D_A = D_MIX // 2
D_B = D_MIX - D_A
D_IN_PROJ = 3 * D_A + 2 * D_B
K_SHORT = 3
K_CONF = 31
N_EXPERTS = 64
TOP_K = 8
N_EXPERT_GROUPS = 8
TOPK_GROUPS = 4
D_EXPERT = D_MODEL // 4
D_SHARED = D_EXPERT
ROUTED_SCALE = 2.5
DISPATCH_BLOCK = 256
EPS = 1e-6

kernel_name = "hybrid_shortconv_conformer_moe_adaln"


def _rmsnorm(x, g):
    xf = x.astype(jnp.float32)
    y = xf * lax.rsqrt(jnp.mean(xf * xf, axis=-1, keepdims=True) + EPS)
    return (y * g.astype(jnp.float32)).astype(x.dtype)


def _head_rmsnorm(y, g):
    shp = y.shape
    yf = y.astype(jnp.float32).reshape(shp[:-1] + (shp[-1] // HEAD_CH, HEAD_CH))
    yf = yf * lax.rsqrt(jnp.mean(yf * yf, axis=-1, keepdims=True) + EPS)
    return (yf.reshape(shp) * g.astype(jnp.float32)).astype(y.dtype)


def _layernorm(x, g, b):
    xf = x.astype(jnp.float32)
    mu = jnp.mean(xf, axis=-1, keepdims=True)
    var = jnp.mean(jnp.square(xf - mu), axis=-1, keepdims=True)
    y = (xf - mu) * lax.rsqrt(var + EPS)
    return (y * g.astype(jnp.float32) + b.astype(jnp.float32)).astype(x.dtype)


def _causal_depthwise_conv(u, w):
    k, ch = w.shape
    return lax.conv_general_dilated(
        u, w[:, None, :].astype(u.dtype), window_strides=(1,), padding=[(k - 1, 0)],
        dimension_numbers=("NWC", "WIO", "NWC"), feature_group_count=ch)


def _modulate(h, shift, scale):
    return h * (1.0 + scale[:, None, :]) + shift[:, None, :]


def _mixer(h, w_in, conv_a_w, conv_b_w, conv_b_b, ln_b_g, ln_b_b, head_norm_a_g, head_norm_b_g, w_out):
    proj = jnp.einsum("bsd,de->bse", h, w_in)
    xa, ba, ca, vb, gb = jnp.split(proj, [D_A, 2 * D_A, 3 * D_A, 3 * D_A + D_B], axis=-1)
    ya = ba * _causal_depthwise_conv(ca * xa, conv_a_w)
    glu = vb * jax.nn.sigmoid(gb)
    zb = _causal_depthwise_conv(glu, conv_b_w) + conv_b_b
    zb = jax.nn.silu(_layernorm(zb, ln_b_g, ln_b_b))
    y = jnp.concatenate([_head_rmsnorm(ya, head_norm_a_g), _head_rmsnorm(zb, head_norm_b_g)], axis=-1)
    return jnp.einsum("bse,ed->bsd", y, w_out)


def _swiglu(x, wg, wu, wd):
    return (jax.nn.silu(x @ wg) * (x @ wu)) @ wd


def _moe(h, w_router, router_bias, w_gate, w_up, w_down, ws_gate, ws_up, ws_down):
    bsz, seq, d = h.shape
    n_tok = bsz * seq
    hf = h.reshape(n_tok, d)
    scores = jax.nn.sigmoid(hf.astype(jnp.float32) @ w_router.astype(jnp.float32))
    biased = scores + router_bias.astype(jnp.float32)
    grp = biased.reshape(n_tok, N_EXPERT_GROUPS, N_EXPERTS // N_EXPERT_GROUPS)
    grp_score = lax.top_k(grp, 2)[0].sum(-1)
    _, top_grp = lax.top_k(grp_score, TOPK_GROUPS)
    grp_mask = jnp.any(top_grp[:, :, None] == jnp.arange(N_EXPERT_GROUPS)[None, None, :], axis=1)
    exp_mask = jnp.repeat(grp_mask, N_EXPERTS // N_EXPERT_GROUPS, axis=1)
    _, idx = lax.top_k(jnp.where(exp_mask, biased, -jnp.inf), TOP_K)
    wts = jnp.take_along_axis(scores, idx, axis=1)
    wts = (wts / jnp.sum(wts, axis=-1, keepdims=True) * ROUTED_SCALE).astype(h.dtype)

    n_assign = n_tok * TOP_K
    n_blocks = (n_assign + N_EXPERTS * (DISPATCH_BLOCK - 1) + DISPATCH_BLOCK - 1) // DISPATCH_BLOCK
    flat_e = idx.reshape(-1)
    flat_tok = jnp.arange(n_assign, dtype=jnp.int32) // TOP_K
    flat_w = wts.reshape(-1)
    order = jnp.argsort(flat_e)
    e_sorted = flat_e[order]
    counts = jnp.bincount(flat_e, length=N_EXPERTS)
    start = jnp.cumsum(counts) - counts
    padded = (counts + DISPATCH_BLOCK - 1) // DISPATCH_BLOCK * DISPATCH_BLOCK
    pend = jnp.cumsum(padded)
    pstart = pend - padded
    dest = pstart[e_sorted] + jnp.arange(n_assign) - start[e_sorted]
    n_rows = n_blocks * DISPATCH_BLOCK
    buf_tok = jnp.zeros((n_rows,), jnp.int32).at[dest].set(flat_tok[order])
    buf_w = jnp.zeros((n_rows,), h.dtype).at[dest].set(flat_w[order])
    block_e = jnp.minimum(
        jnp.searchsorted(pend, jnp.arange(n_blocks) * DISPATCH_BLOCK, side="right"), N_EXPERTS - 1)

    def body(acc, blk):
        tok, wt, e = blk
        yb = _swiglu(hf[tok], w_gate[e], w_up[e], w_down[e])
        return acc.at[tok].add(yb * wt[:, None]), None

    routed, _ = lax.scan(body, jnp.zeros_like(hf),
                         (buf_tok.reshape(n_blocks, DISPATCH_BLOCK),
                          buf_w.reshape(n_blocks, DISPATCH_BLOCK), block_e))
    shared = _swiglu(hf, ws_gate, ws_up, ws_down)
    return (routed + shared).reshape(bsz, seq, d)


def setup_inputs(seed: int = 0) -> dict:
    key = jax.random.key(seed)
    ks = jax.random.split(key, 24)
    L, D, E, F = DEPTH, D_MODEL, N_EXPERTS, D_EXPERT
    nrm = lambda k, shp, s: jax.random.normal(k, shp, jnp.float32) * s
    gain = lambda k, shp: 1.0 + 0.02 * jax.random.normal(k, shp, jnp.float32)
    return {
        "x": nrm(ks[0], (BATCH, SEQ, D), 1.0),
        "c": nrm(ks[1], (BATCH, D), 1.0),
        "w_ada": nrm(ks[2], (L, D, 6 * D), 0.5 * D ** -0.5),
        "b_ada": nrm(ks[3], (L, 6 * D), 0.02),
        "norm_mix_g": gain(ks[4], (L, D)),
        "w_in": nrm(ks[5], (L, D, D_IN_PROJ), D ** -0.5),
        "conv_a_w": nrm(ks[6], (L, K_SHORT, D_A), K_SHORT ** -0.5),
        "conv_b_w": nrm(ks[7], (L, K_CONF, D_B), K_CONF ** -0.5),
        "conv_b_b": nrm(ks[8], (L, D_B), 0.02),
        "ln_b_g": gain(ks[9], (L, D_B)),
        "ln_b_b": nrm(ks[10], (L, D_B), 0.02),
        "head_norm_a_g": gain(ks[11], (L, D_A)),
        "head_norm_b_g": gain(ks[12], (L, D_B)),
        "w_out": nrm(ks[13], (L, D_MIX, D), D_MIX ** -0.5),
        "norm_ffn_g": gain(ks[14], (L, D)),
        "w_router": nrm(ks[15], (L, D, E), D ** -0.5),
        "router_bias": nrm(ks[16], (L, E), 0.01),
        "w_gate": nrm(ks[17], (L, E, D, F), D ** -0.5),
        "w_up": nrm(ks[18], (L, E, D, F), D ** -0.5),
        "w_down": nrm(ks[19], (L, E, F, D), F ** -0.5),
        "w_shared_gate": nrm(ks[20], (L, D, D_SHARED), D ** -0.5),
        "w_shared_up": nrm(ks[21], (L, D, D_SHARED), D ** -0.5),
        "w_shared_down": nrm(ks[22], (L, D_SHARED, D), D_SHARED ** -0.5),
        "norm_final_g": gain(ks[23], (D,)),
    }


def reference(x, c, w_ada, b_ada, norm_mix_g, w_in, conv_a_w, conv_b_w, conv_b_b, ln_b_g, ln_b_b,
              head_norm_a_g, head_norm_b_g, w_out, norm_ffn_g, w_router, router_bias, w_gate, w_up,
              w_down, w_shared_gate, w_shared_up, w_shared_down, norm_final_g):
    c_act = jax.nn.silu(c)
    for l in range(DEPTH):
        mod = c_act @ w_ada[l] + b_ada[l]
        sh_m, sc_m, g_m, sh_f, sc_f, g_f = jnp.split(mod, 6, axis=-1)
        h = _modulate(_rmsnorm(x, norm_mix_g[l]), sh_m, sc_m)
        x = x + g_m[:, None, :] * _mixer(h, w_in[l], conv_a_w[l], conv_b_w[l], conv_b_b[l], ln_b_g[l],
                                         ln_b_b[l], head_norm_a_g[l], head_norm_b_g[l], w_out[l])
        h = _modulate(_rmsnorm(x, norm_ffn_g[l]), sh_f, sc_f)
        x = x + g_f[:, None, :] * _moe(h, w_router[l], router_bias[l], w_gate[l], w_up[l], w_down[l],
                                       w_shared_gate[l], w_shared_up[l], w_shared_down[l])
    return _rmsnorm(x, norm_final_g)
```

```python
import os
import numpy as np
from contextlib import ExitStack
import concourse.bass as bass
import concourse.mybir as mybir
from concourse.bass_utils import run_bass_kernel_spmd

F32 = mybir.dt.float32
BF16 = mybir.dt.bfloat16
I32 = mybir.dt.int32
ALU = mybir.AluOpType
AF = mybir.ActivationFunctionType
AX = mybir.AxisListType

D = 2048
KC = 16
NE = 64
FE = 512
EPS = 1e-6
NCORES = 8

ENGS = ("pe", "act", "dve", "pool", "sp")


class Buf:
    __slots__ = ("name", "t", "lw", "rd")

    def __init__(self, name, t=None):
        self.name = name
        self.t = t
        self.lw = None
        self.rd = []

    def __getitem__(self, k):
        return self.t[k]


class Prog:
    def __init__(self, nc, n_dma_sems=12):
        self.nc = nc
        self.es = ExitStack()
        self.eng = {"pe": nc.tensor, "act": nc.scalar, "dve": nc.vector,
                    "pool": nc.gpsimd, "sp": nc.sync}
        self.ops = {e: [] for e in ENGS}
        self.waited = {e: {} for e in ENGS}
        self.psem = {}
        self.pcnt = {}
        self.n_dma_sems = n_dma_sems
        self.dsem = {}
        self.dcnt = {}
        self.dlast = {}
        self.drr = {}
        self.last_ev = {e: None for e in ENGS}
        self.gen = 0
        for q in ("sp", "pool", "act"):
            self.dsem[q] = [self._sem(f"d_{q}{i}") for i in range(n_dma_sems)]
            self.dcnt[q] = [0] * n_dma_sems
            self.dlast[q] = [None] * n_dma_sems
            self.drr[q] = 0
        self.new_phase_sems()

    def _sem(self, name):
        return self.es.enter_context(self.nc.semaphore(name))

    def new_phase_sems(self):
        self.gen += 1
        for e in ("pe", "act", "dve", "pool"):
            self.psem[e] = self._sem(f"p{self.gen}_{e}")
            self.pcnt[e] = 0

    def _need(self, e, evs):
        w = self.waited[e]
        best = {}
        for ev in evs:
            if ev is None:
                continue
            s, v = ev
            if w.get(id(s), 0) >= v:
                continue
            if id(s) not in best or best[id(s)][1] < v:
                best[id(s)] = (s, v)
        out = []
        for k, (s, v) in best.items():
            w[k] = v
            out.append((s, v))
        return out

    @staticmethod
    def _deps(reads, writes):
        evs = []
        for b in reads:
            evs.append(b.lw)
        for b in writes:
            evs.append(b.lw)
            evs.extend(b.rd)
        return evs

    @staticmethod
    def _commit(ev, reads, writes):
        for b in reads:
            b.rd.append(ev)
        for b in writes:
            b.lw = ev
            b.rd = []

    def op(self, e, fn, reads=(), writes=(), extra=()):
        evs = self._deps(reads, writes) + list(extra)
        waits = self._need(e, evs)
        self.pcnt[e] += 1
        ev = (self.psem[e], self.pcnt[e])
        self.ops[e].append((waits, fn, ev, 1))
        self._commit(ev, reads, writes)
        self.last_ev[e] = ev
        return ev

    def dma(self, q, fn, reads=(), writes=(), extra=()):
        i = self.drr[q]
        self.drr[q] = (i + 1) % self.n_dma_sems
        evs = self._deps(reads, writes) + list(extra) + [self.dlast[q][i]]
        waits = self._need(q, evs)
        self.dcnt[q][i] += 16
        ev = (self.dsem[q][i], self.dcnt[q][i])
        self.dlast[q][i] = ev
        self.ops[q].append((waits, fn, ev, 16))
        self._commit(ev, reads, writes)
        return ev

    def barrier(self):
        evs = [self.last_ev[e] for e in ("pe", "act", "dve", "pool")]
        for q in ("sp", "pool", "act"):
            evs.extend(self.dlast[q])
        for e in ENGS:
            waits = self._need(e, evs)
            if waits:
                self.ops[e].append((waits, None, None, 0))

    def flush(self, final=False):
        if final:
            self.barrier()
        ops = self.ops

        def emit(e):
            def body(engine):
                for waits, fn, ev, inc in ops[e]:
                    for s, v in waits:
                        engine.wait_ge(s, v)
                    if fn is not None:
                        ins = fn(engine)
                        ins.then_inc(ev[0], inc)
            return body

        with self.nc.Block() as block:
            block.tensor(emit("pe"))
            block.scalar(emit("act"))
            block.vector(emit("dve"))
            block.gpsimd(emit("pool"))
            block.sync(emit("sp"))
        self.ops = {e: [] for e in ENGS}


FA_W = 0
FB_W = 24
FB_B = 24 + 248
FLN_G = FB_B + 8
FLN_B = FLN_G + 8
FHA = FLN_B + 8
FHB = FHA + 8
NF = FHB + 8
R_GMIX = 0
R_GFFN = D
R_GFIN = 2 * D
R_RB = 3 * D
R_BADA = 3 * D + NE
NR = R_BADA + 6 * D


def build(NB, S, C, phases=5, dbg=False):
    TC = NB * S
    NT = TC // 128
    NT5 = TC // 512
    T5B = S // 512
    NSLOT = NE * C
    groups = []
    r = 0
    while r < C:
        n = min(512, C - r)
        groups.append((r, n))
        r += n

    nc = bass.Bass("TRN2", target_bir_lowering=False)
    dt_in = lambda name, shape: nc.dram_tensor(name, shape, F32, kind="ExternalInput").ap()
    x_d = dt_in("x", [TC, D])
    cT_d = dt_in("cT", [128, KC, NB])
    wada_d = dt_in("w_ada", [128, KC, 6 * D])
    rvec_d = dt_in("rvec", [1, NR])
    fvec_d = dt_in("fvec", [128, NF])
    win_d = dt_in("w_in", [8, 128, KC, 640])
    wout_d = dt_in("w_out", [128, KC, D])
    wr_d = dt_in("w_router", [128, KC, NE])
    wsg_d = dt_in("ws_gate", [128, KC, FE])
    wsu_d = dt_in("ws_up", [128, KC, FE])
    wsd_d = dt_in("ws_down", [128, 4, D])
    wg_d = dt_in("w_gate", [NE, 128, KC, FE])
    wu_d = dt_in("w_up", [NE, 128, KC, FE])
    wd_d = dt_in("w_down", [NE, 128, 4, D])
    out_d = nc.dram_tensor("out", [TC, D], F32, kind="ExternalOutput").ap()
    sk = "ExternalOutput" if dbg else "Internal"
    mod_d = nc.dram_tensor("modd", [NB, 6 * D], F32, kind=sk).ap()
    yT_d = nc.dram_tensor("yTd", [NT5, 128, KC, 512], BF16, kind=sk).ap()
    x1_d = nc.dram_tensor("x1d", [TC, D], F32, kind=sk).ap()
    x2_d = nc.dram_tensor("x2d", [TC, D], F32, kind=sk).ap()
    xb_d = nc.dram_tensor("xbuf", [NSLOT, D], BF16, kind="Internal").ap()
    yb_d = nc.dram_tensor("ybuf", [NSLOT, D], BF16, kind="Internal").ap()
    if dbg:
        dbg_d = nc.dram_tensor("dbg", [TC, 16], F32, kind="ExternalOutput").ap()
    B_mod, B_yT, B_x1, B_x2, B_xb, B_yb, B_out = (Buf(n) for n in ("modd", "yTd", "x1d", "x2d", "xbuf", "ybuf", "outd"))

    P = Prog(nc)
    top = ExitStack()
    bc_reg = {}

    def get_bc(e, key):
        if key not in bc_reg:
            r_ = e.alloc_register(f"bc{key}")
            e.reg_mov(r_, NSLOT - 1)
            bc_reg[key] = r_
        return bc_reg[key]

    def mk(es, name, shape, dt):
        return Buf(name, es.enter_context(nc.sbuf_tensor("s_" + name, shape, dt)))

    def mkp(es, name, shape, dt):
        return Buf(name, es.enter_context(nc.psum_tensor("q_" + name, shape, dt)))

    posall = mk(top, "posall", [128, NT * 8], I32)
    wall = mk(top, "wall", [128, NT * 8], F32)
    ident = mk(top, "ident", [128, 128], BF16)
    epsT = mk(top, "epsT", [128, 1], F32)
    fvec = mk(top, "fvec", [128, NF], F32)

    with ExitStack() as es:
        identf = mk(es, "identf", [128, 128], F32)
        cT = mk(es, "cT", [128, KC, NB], F32)
        cact = mk(es, "cact", [128, KC, NB], F32)
        wa = [mk(es, f"wa{i}", [128, KC, 512], F32) for i in range(2)]
        modsb = mk(es, "modsb", [NB, 6 * D], F32)
        badaB = mk(es, "badaB", [NB, 6 * D], F32)
        pm = [mkp(es, f"pm{i}", [128, 512], F32) for i in range(2)]
        zt = mk(es, "zt", [128, 8192], BF16)

        P.op("pool", lambda e: e.memset(identf[:, :], 0.0), writes=[identf])
        P.op("pool", lambda e: e.affine_select(out=identf[:, :], in_=identf[:, :], pattern=[[-1, 128]],
                                               compare_op=ALU.not_equal, fill=1.0, base=0, channel_multiplier=1),
             reads=[identf], writes=[identf])
        P.op("dve", lambda e: e.tensor_copy(out=ident[:, :], in_=identf[:, :]), reads=[identf], writes=[ident])
        P.op("dve", lambda e: e.memset(epsT[:, :], EPS), writes=[epsT])
        P.op("dve", lambda e: e.memset(wall[:, :], 0.0), writes=[wall])
        P.op("pool", lambda e: e.memset(zt[:, :], 0.0), writes=[zt])
        P.dma("sp", lambda e: e.dma_start(out=fvec[:, :], in_=fvec_d[:, :]), writes=[fvec])
        P.dma("sp", lambda e: e.dma_start(out=cT[:, :, :], in_=cT_d[:, :, :]), writes=[cT])
        P.dma("sp", lambda e: e.dma_start(out=badaB[:, :], in_=rvec_d[0:1, R_BADA:R_BADA + 6 * D].partition_broadcast(NB)),
              writes=[badaB])
        P.op("act", lambda e: e.activation(out=cact[:, :, :], in_=cT[:, :, :], func=AF.Silu), reads=[cT], writes=[cact])
        xb_flat = xb_d.rearrange("(a p r) d -> a p (r d)", p=128, r=4)
        for a in range(NSLOT // 512):
            P.dma("pool", lambda e, a=a: e.dma_start(out=xb_flat[a], in_=zt[:, :]), reads=[zt], writes=[B_xb])
        for nb in range(24):
            w = wa[nb % 2]
            P.dma("sp", lambda e, nb=nb, w=w: e.dma_start(out=w[:, :, :], in_=wada_d[:, :, nb * 512:(nb + 1) * 512]), writes=[w])
            pp = pm[nb % 2]

            def mm(e, w=w, pp=pp):
                for kc in range(KC):
                    ins = e.matmul(pp[0:NB, :], lhsT=cact[:, kc, :], rhs=w[:, kc, :], start=(kc == 0), stop=(kc == KC - 1))
                return ins
            P.op("pe", mm, reads=[cact, w], writes=[pp])
            P.op("dve", lambda e, nb=nb, pp=pp: e.tensor_tensor(out=modsb[:, nb * 512:(nb + 1) * 512], in0=pp[0:NB, :],
                                                                 in1=badaB[:, nb * 512:(nb + 1) * 512], op=ALU.add),
                 reads=[pp, badaB], writes=[modsb])
        P.dma("sp", lambda e: e.dma_start(out=mod_d[:, :], in_=modsb[:, :]), reads=[modsb], writes=[B_mod])
        P.flush()

    def bcast_load(q, dst, src_ap, nparts=128, reads=()):
        return P.dma(q, lambda e: e.dma_start(out=dst[:, :], in_=src_ap.partition_broadcast(nparts)), reads=list(reads), writes=[dst])

    def rms_front(es_bufs, xt, m_scale, m_shift, hout):
        junk, ssq, rstd, hn = es_bufs
        P.op("act", lambda e: e.activation(out=junk[:, :], in_=xt[:, :], func=AF.Square, accum_out=ssq[:, 0:1]),
             reads=[xt], writes=[junk, ssq])
        P.op("act", lambda e: e.activation(out=rstd[:, 0:1], in_=ssq[:, 0:1], func=AF.Sqrt, bias=epsT[:, 0:1], scale=1.0 / D),
             reads=[ssq, epsT], writes=[rstd])
        P.op("dve", lambda e: e.reciprocal(out=rstd[:, 0:1], in_=rstd[:, 0:1]), reads=[rstd], writes=[rstd])
        P.op("dve", lambda e: e.scalar_tensor_tensor(out=hn[:, :], in0=xt[:, :], scalar=rstd[:, 0:1], in1=m_scale[:, :],
                                                     op0=ALU.mult, op1=ALU.mult), reads=[xt, rstd, m_scale], writes=[hn])
        P.op("pool", lambda e: e.tensor_tensor(out=hout[:, :], in0=hn[:, :], in1=m_shift[:, :], op=ALU.add),
             reads=[hn, m_shift], writes=[hout])

    def transposes(src, pT, dstT, s, evac_engs):
        for h in range(2):
            pb = pT[h]

            def tr(e, h=h, pb=pb):
                for k in range(8):
                    kc = h * 8 + k
                    ins = e.transpose(out=pb[:, k * 128:(k + 1) * 128], in_=src[:, kc * 128:(kc + 1) * 128], identity=ident[:, :])
                return ins
            P.op("pe", tr, reads=[src, ident], writes=[pb])
            en = evac_engs[h]
            o_ap = dstT[:, h * 8:(h + 1) * 8, s * 128:(s + 1) * 128]
            i_ap = pb[:, :].rearrange("p (k t) -> p k t", k=8)
            if en == "act":
                P.op("act", lambda e, o_ap=o_ap, i_ap=i_ap: e.copy(out=o_ap, in_=i_ap), reads=[pb], writes=[dstT])
            else:
                P.op(en, lambda e, o_ap=o_ap, i_ap=i_ap: e.tensor_copy(out=o_ap, in_=i_ap), reads=[pb], writes=[dstT])

    if phases < 1:
        P.flush(final=True)
        top.close()
        return nc

    P.new_phase_sems()
    with ExitStack() as es:
        P.barrier()
        m1B = mk(es, "m1B", [128, D], F32)
        shmB = mk(es, "shmB", [128, D], F32)
        gmixB = mk(es, "gmixB", [128, D], F32)
        xs = [mk(es, f"xs{i}", [128, D], F32) for i in range(2)]
        hn = mk(es, "hn", [128, D], F32)
        junk = mk(es, "junk", [128, D], BF16)
        htm = [mk(es, f"htm{i}", [128, D], BF16) for i in range(2)]
        ssq = [mk(es, f"ssq{i}", [128, 1], F32) for i in range(2)]
        rstd = [mk(es, f"rstd{i}", [128, 1], F32) for i in range(2)]
        hT = mk(es, "hT", [128, KC, 512], BF16)
        win = [mk(es, f"win{i}", [128, KC, 640], BF16) for i in range(2)]
        yT = mk(es, "yT", [128, KC, 512], BF16)
        uhist = mk(es, "uhist", [128, 8, 2], F32)
        ghist = mk(es, "ghist", [128, 8, 30], F32)
        uw = [mk(es, f"uw{i}", [128, 514], F32) for i in range(2)]
        gw = [mk(es, f"gw{i}", [128, 542], F32) for i in range(2)]
        zb = [mk(es, f"zb{j}", [128, 512], F32) for j in range(8)]
        xa_s = [mk(es, f"xa_s{i}", [128, 512], F32) for i in range(2)]
        acc_a = [mk(es, f"acc_a{i}", [128, 512], F32) for i in range(2)]
        ya = [mk(es, f"ya{i}", [128, 512], F32) for i in range(2)]
        sq = [mk(es, f"sq{i}", [128, 512], F32) for i in range(2)]
        rs = [mk(es, f"rs{i}", [128, 512], F32) for i in range(2)]
        sg = [mk(es, f"sg{i}", [128, 512], F32) for i in range(2)]
        mean = mk(es, "mean", [128, 512], F32)
        msq = mk(es, "msq", [128, 512], F32)
        lrstd = mk(es, "lrstd", [128, 512], F32)
        t1 = [mk(es, f"t1{i}", [128, 512], F32) for i in range(2)]
        zz = [mk(es, f"zz{i}", [128, 512], F32) for i in range(2)]
        onesf = mk(es, "onesf", [128, 128], F32)
        blkf = mk(es, "blkf", [128, 128], F32)
        pT = [mkp(es, f"pT{i}", [128, 1024], BF16) for i in range(2)]
        pr = [mkp(es, f"pr{i}", [128, 512], F32) for i in range(3)]
        pS1 = mkp(es, "pS1", [128, 512], F32)
        pS2 = mkp(es, "pS2", [128, 512], F32)
        ph = mkp(es, "ph", [128, 512], F32)
        prr = [0]

        def next_pr():
            b = pr[prr[0] % 3]
            prr[0] += 1
            return b

        P.op("dve", lambda e: e.memset(onesf[:, :], 1.0), writes=[onesf])
        P.op("pool", lambda e: e.memset(blkf[:, :], 0.0), writes=[blkf])
        P.op("pool", lambda e: e.memset(blkf[0:64, 0:64], 1.0), reads=[blkf], writes=[blkf])
        P.op("pool", lambda e: e.memset(blkf[64:128, 64:128], 1.0), reads=[blkf], writes=[blkf])
        bcast_load("sp", gmixB, rvec_d[0:1, R_GMIX:R_GMIX + D])

        def head_norm(src, gcol, dst_ap, dst_buf, i2):
            P.op("act", lambda e: e.activation(out=sq[i2][:, :], in_=src[:, :], func=AF.Square), reads=[src], writes=[sq[i2]])
            P.op("pe", lambda e: e.matmul(ph[:, :], lhsT=blkf[:, :], rhs=sq[i2][:, :], start=True, stop=True),
                 reads=[blkf, sq[i2]], writes=[ph])
            P.op("act", lambda e: e.activation(out=rs[i2][:, :], in_=ph[:, :], func=AF.Sqrt, bias=epsT[:, 0:1], scale=1.0 / 64),
                 reads=[ph, epsT], writes=[rs[i2]])
            P.op("dve", lambda e: e.reciprocal(out=rs[i2][:, :], in_=rs[i2][:, :]), reads=[rs[i2]], writes=[rs[i2]])
            P.op("dve", lambda e: e.scalar_tensor_tensor(out=dst_ap, in0=src[:, :], scalar=fvec[:, gcol:gcol + 1], in1=rs[i2][:, :],
                                                         op0=ALU.mult, op1=ALU.mult), reads=[src, fvec, rs[i2]], writes=[dst_buf])

        jcount = 0
        for tt in range(NT5):
            b, ti = divmod(tt, T5B)
            if ti == 0:
                bcast_load("sp", m1B, mod_d[b:b + 1, D:2 * D], reads=[B_mod])
                P.op("dve", lambda e: e.scalar_tensor_tensor(out=m1B[:, :], in0=m1B[:, :], scalar=1.0, in1=gmixB[:, :],
                                                             op0=ALU.add, op1=ALU.mult), reads=[m1B, gmixB], writes=[m1B])
                bcast_load("sp", shmB, mod_d[b:b + 1, 0:D], reads=[B_mod])
                P.op("dve", lambda e: e.memset(uhist[:, :, :], 0.0), reads=[uhist], writes=[uhist])
                P.op("dve", lambda e: e.memset(ghist[:, :, :], 0.0), reads=[ghist], writes=[ghist])
            for s in range(4):
                i2 = s % 2
                xt = xs[i2]
                r0 = tt * 512 + s * 128
                P.dma("sp", lambda e, xt=xt, r0=r0: e.dma_start(out=xt[:, :], in_=x_d[r0:r0 + 128, :]), writes=[xt])
                rms_front((junk, ssq[i2], rstd[i2], hn), xt, m1B, shmB, htm[i2])
                transposes(htm[i2], pT, hT, s, ("act", "dve"))
            for j in range(8):
                wj = win[jcount % 2]
                jcount += 1
                P.dma("pool", lambda e, wj=wj, j=j: e.dma_start(out=wj[:, :, :], in_=win_d[j]), writes=[wj])

                def proj(c, wj=wj):
                    pb = next_pr()

                    def mm(e, pb=pb, c=c, wj=wj):
                        for kc in range(KC):
                            ins = e.matmul(pb[:, :], lhsT=wj[:, kc, c * 128:(c + 1) * 128], rhs=hT[:, kc, :],
                                           start=(kc == 0), stop=(kc == KC - 1))
                        return ins
                    P.op("pe", mm, reads=[wj, hT], writes=[pb])
                    return pb
                i2 = j % 2
                p_xa = proj(0)
                P.op("act", lambda e, p=p_xa, i2=i2: e.copy(out=xa_s[i2][:, :], in_=p[:, :]), reads=[p_xa], writes=[xa_s[i2]])
                p_ba = proj(1)
                p_ca = proj(2)
                u = uw[i2]
                P.op("dve", lambda e, u=u, j=j: e.tensor_copy(out=u[:, 0:2], in_=uhist[:, j, :]), reads=[uhist], writes=[u])
                P.op("dve", lambda e, u=u, p=p_ca, i2=i2: e.tensor_tensor(out=u[:, 2:514], in0=p[:, :], in1=xa_s[i2][:, :], op=ALU.mult),
                     reads=[p_ca, xa_s[i2]], writes=[u])
                P.op("dve", lambda e, u=u, j=j: e.tensor_copy(out=uhist[:, j, :], in_=u[:, 512:514]), reads=[u], writes=[uhist])
                aa = acc_a[i2]
                P.op("dve", lambda e, u=u, aa=aa, j=j: e.tensor_scalar(out=aa[:, :], in0=u[:, 0:512], scalar1=fvec[:, FA_W + j * 3:FA_W + j * 3 + 1],
                                                                       scalar2=None, op0=ALU.mult), reads=[u, fvec], writes=[aa])
                for k in (1, 2):
                    P.op("dve", lambda e, u=u, aa=aa, j=j, k=k: e.scalar_tensor_tensor(
                        out=aa[:, :], in0=u[:, k:k + 512], scalar=fvec[:, FA_W + j * 3 + k:FA_W + j * 3 + k + 1], in1=aa[:, :],
                        op0=ALU.mult, op1=ALU.add), reads=[u, fvec, aa], writes=[aa])
                P.op("dve", lambda e, p=p_ba, aa=aa, i2=i2: e.tensor_tensor(out=ya[i2][:, :], in0=p[:, :], in1=aa[:, :], op=ALU.mult),
                     reads=[p_ba, aa], writes=[ya[i2]])
                head_norm(ya[i2], FHA + j, yT[:, j, :], yT, i2)
                p_vb = proj(3)
                p_gb = proj(4)
                P.op("act", lambda e, p=p_gb, i2=i2: e.activation(out=sg[i2][:, :], in_=p[:, :], func=AF.Sigmoid), reads=[p_gb], writes=[sg[i2]])
                g = gw[i2]
                ceng = "dve"
                P.op(ceng, lambda e, g=g, j=j: e.tensor_copy(out=g[:, 0:30], in_=ghist[:, j, :]), reads=[ghist], writes=[g])
                P.op("dve", lambda e, g=g, p=p_vb, i2=i2: e.tensor_tensor(out=g[:, 30:542], in0=p[:, :], in1=sg[i2][:, :], op=ALU.mult),
                     reads=[p_vb, sg[i2]], writes=[g])
                P.op(ceng, lambda e, g=g, j=j: e.tensor_copy(out=ghist[:, j, :], in_=g[:, 512:542]), reads=[g], writes=[ghist])
                z = zb[j]
                P.op(ceng, lambda e, g=g, z=z, j=j: e.tensor_scalar(out=z[:, :], in0=g[:, 0:512], scalar1=fvec[:, FB_W + j * 31:FB_W + j * 31 + 1],
                                                                    scalar2=fvec[:, FB_B + j:FB_B + j + 1], op0=ALU.mult, op1=ALU.add),
                     reads=[g, fvec], writes=[z])
                for k in range(1, 31):
                    P.op(ceng, lambda e, g=g, z=z, j=j, k=k: e.scalar_tensor_tensor(
                        out=z[:, :], in0=g[:, k:k + 512], scalar=fvec[:, FB_W + j * 31 + k:FB_W + j * 31 + k + 1], in1=z[:, :],
                        op0=ALU.mult, op1=ALU.add), reads=[g, fvec, z], writes=[z])
                P.op("act", lambda e, z=z, i2=i2: e.activation(out=sq[i2][:, :], in_=z[:, :], func=AF.Square), reads=[z], writes=[sq[i2]])
                P.op("pe", lambda e, z=z, j=j: e.matmul(pS1[:, :], lhsT=onesf[:, :], rhs=z[:, :], start=(j == 0), stop=(j == 7)),
                     reads=[onesf, z], writes=[pS1])
                P.op("pe", lambda e, i2=i2, j=j: e.matmul(pS2[:, :], lhsT=onesf[:, :], rhs=sq[i2][:, :], start=(j == 0), stop=(j == 7)),
                     reads=[onesf, sq[i2]], writes=[pS2])
            P.op("act", lambda e: e.activation(out=mean[:, :], in_=pS1[:, :], func=AF.Identity, scale=1.0 / 1024), reads=[pS1], writes=[mean])
            P.op("dve", lambda e: e.tensor_tensor(out=msq[:, :], in0=mean[:, :], in1=mean[:, :], op=ALU.mult), reads=[mean], writes=[msq])
            P.op("dve", lambda e: e.scalar_tensor_tensor(out=msq[:, :], in0=pS2[:, :], scalar=1.0 / 1024, in1=msq[:, :],
                                                         op0=ALU.mult, op1=ALU.subtract), reads=[pS2, msq], writes=[msq])
            P.op("act", lambda e: e.activation(out=lrstd[:, :], in_=msq[:, :], func=AF.Sqrt, bias=epsT[:, 0:1], scale=1.0),
                 reads=[msq, epsT], writes=[lrstd])
            P.op("dve", lambda e: e.reciprocal(out=lrstd[:, :], in_=lrstd[:, :]), reads=[lrstd], writes=[lrstd])
            for j in range(8):
                i2 = j % 2
                z = zb[j]
                P.op("dve", lambda e, z=z, i2=i2: e.tensor_tensor(out=t1[i2][:, :], in0=z[:, :], in1=mean[:, :], op=ALU.subtract),
                     reads=[z, mean], writes=[t1[i2]])
                P.op("pool", lambda e, i2=i2: e.tensor_tensor(out=t1[i2][:, :], in0=t1[i2][:, :], in1=lrstd[:, :], op=ALU.mult),
                     reads=[t1[i2], lrstd], writes=[t1[i2]])
                P.op("act", lambda e, i2=i2, j=j: e.activation(out=zz[i2][:, :], in_=t1[i2][:, :], func=AF.Silu,
                                                               scale=fvec[:, FLN_G + j:FLN_G + j + 1], bias=fvec[:, FLN_B + j:FLN_B + j + 1]),
                     reads=[t1[i2], fvec], writes=[zz[i2]])
                head_norm(zz[i2], FHB + j, yT[:, 8 + j, :], yT, i2)
            P.dma("sp", lambda e, tt=tt: e.dma_start(out=yT_d[tt], in_=yT[:, :, :]), reads=[yT], writes=[B_yT])
        P.flush()

    if phases < 2:
        P.barrier()
        P.flush(final=True)
        top.close()
        return nc

    P.new_phase_sems()
    with ExitStack() as es:
        P.barrier()
        wo = mk(es, "wo", [128, KC, D], BF16)
        gmB = mk(es, "gmB", [128, D], F32)
        yTs = [mk(es, f"yTs{i}", [128, KC, 512], BF16) for i in range(2)]
        xr = [mk(es, f"xr{i}", [128, D], F32) for i in range(3)]
        tmpo = [mk(es, f"tmpo{i}", [128, 512], F32) for i in range(2)]
        po = [mkp(es, f"po{i}", [128, 512], F32) for i in range(4)]
        for q in range(4):
            P.dma("pool", lambda e, q=q: e.dma_start(out=wo[:, q * 4:(q + 1) * 4, :], in_=wout_d[:, q * 4:(q + 1) * 4, :]), writes=[wo])
        cnt = 0
        for tt in range(NT5):
            b, ti = divmod(tt, T5B)
            if ti == 0:
                bcast_load("sp", gmB, mod_d[b:b + 1, 2 * D:3 * D], reads=[B_mod])
            yt = yTs[tt % 2]
            P.dma("sp", lambda e, yt=yt, tt=tt: e.dma_start(out=yt[:, :, :], in_=yT_d[tt]), reads=[B_yT], writes=[yt])
            for s in range(4):
                xt = xr[(tt * 4 + s) % 3]
                r0 = tt * 512 + s * 128
                P.dma("sp", lambda e, xt=xt, r0=r0: e.dma_start(out=xt[:, :], in_=x_d[r0:r0 + 128, :]), writes=[xt])
                for n in range(4):
                    pb = po[cnt % 4]
                    tm = tmpo[cnt % 2]
                    cnt += 1

                    def mm(e, pb=pb, yt=yt, s=s, n=n):
                        for kc in range(KC):
                            ins = e.matmul(pb[:, :], lhsT=yt[:, kc, s * 128:(s + 1) * 128], rhs=wo[:, kc, n * 512:(n + 1) * 512],
                                           start=(kc == 0), stop=(kc == KC - 1))
                        return ins
                    P.op("pe", mm, reads=[yt, wo], writes=[pb])
                    P.op("dve", lambda e, pb=pb, tm=tm, n=n: e.tensor_tensor(out=tm[:, :], in0=pb[:, :], in1=gmB[:, n * 512:(n + 1) * 512], op=ALU.mult),
                         reads=[pb, gmB], writes=[tm])
                    P.op("pool", lambda e, tm=tm, xt=xt, n=n: e.tensor_tensor(out=xt[:, n * 512:(n + 1) * 512], in0=tm[:, :],
                                                                               in1=xt[:, n * 512:(n + 1) * 512], op=ALU.add),
                         reads=[tm, xt], writes=[xt])
                P.dma("sp", lambda e, xt=xt, r0=r0: e.dma_start(out=x1_d[r0:r0 + 128, :], in_=xt[:, :]), reads=[xt], writes=[B_x1])
        P.flush()

    if phases < 3:
        P.barrier()
        P.flush(final=True)
        top.close()
        return nc

    P.new_phase_sems()
    with ExitStack() as es:
        P.barrier()
        m2B = mk(es, "m2B", [128, D], F32)
        shfB = mk(es, "shfB", [128, D], F32)
        gfB = mk(es, "gfB", [128, D], F32)
        x1s = [mk(es, f"x1s{i}", [128, D], F32) for i in range(4)]
        hn = mk(es, "hn2", [128, D], F32)
        junk = mk(es, "junk2", [128, D], BF16)
        h2tm = [mk(es, f"h2tm{i}", [128, D], BF16) for i in range(5)]
        ssq = [mk(es, f"ssq2{i}", [128, 1], F32) for i in range(2)]
        rstd = [mk(es, f"rstd2{i}", [128, 1], F32) for i in range(2)]
        h2T = mk(es, "h2T", [128, KC, 512], BF16)
        wsg = mk(es, "wsg", [128, KC, FE], BF16)
        wsu = mk(es, "wsu", [128, KC, FE], BF16)
        wsd = mk(es, "wsd", [128, 4, D], BF16)
        wrb = mk(es, "wrb", [128, KC, NE], BF16)
        AT = mk(es, "AT", [128, 4, 512], BF16)
        sgs = [mk(es, f"sgs{i}", [128, 512], F32) for i in range(2)]
        tmpo = [mk(es, f"tmp2o{i}", [128, 512], F32) for i in range(2)]
        selall = mk(es, "selall", [128, NT, NE], BF16)
        onesb = mk(es, "onesb", [128, 128], BF16)
        trib = mk(es, "trib", [128, 128], BF16)
        trif = mk(es, "trif", [128, 128], F32)
        rbB = mk(es, "rbB", [128, NE], F32)
        eC1 = mk(es, "eC1", [128, NE], F32)
        eCi = mk(es, "eCi", [128, NE], I32)
        R = {n: mk(es, "r_" + n, [128, NE], F32) for n in ("sc", "bi", "eq", "t", "mk", "masked", "sel", "ws", "lt", "g2", "selv", "pf", "jk")}
        R8 = {n: mk(es, "r8_" + n, [128, 8], F32) for n in ("m1", "m2", "gs", "g8", "gm", "off", "m8", "p8")}
        ssum = mk(es, "ssum", [128, 1], F32)
        pT = [mkp(es, f"pT2{i}", [128, 1024], BF16) for i in range(2)]
        pgu = [mkp(es, f"pgu{i}", [128, 512], F32) for i in range(4)]
        plog = mkp(es, "plog", [128, 512], F32)
        prk = mkp(es, "prk", [128, 512], F32)

        P.dma("pool", lambda e: e.dma_start(out=wsg[:, :, :], in_=wsg_d[:, :, :]), writes=[wsg])
        P.dma("pool", lambda e: e.dma_start(out=wsu[:, :, :], in_=wsu_d[:, :, :]), writes=[wsu])
        P.dma("pool", lambda e: e.dma_start(out=wsd[:, :, :], in_=wsd_d[:, :, :]), writes=[wsd])
        P.dma("pool", lambda e: e.dma_start(out=wrb[:, :, :], in_=wr_d[:, :, :]), writes=[wrb])
        bcast_load("sp", rbB, rvec_d[0:1, R_RB:R_RB + NE])
        P.op("dve", lambda e: e.memset(onesb[:, :], 1.0), writes=[onesb])
        trii = mk(es, "trii", [128, 128], I32)
        P.op("pool", lambda e: e.iota(trii[:, :], pattern=[[1, 128]], base=0, channel_multiplier=-1), writes=[trii])
        P.op("dve", lambda e: e.tensor_copy(out=trif[:, :], in_=trii[:, :]), reads=[trii], writes=[trif])
        P.op("dve", lambda e: e.tensor_scalar(out=trib[:, :], in0=trif[:, :], scalar1=0.0, scalar2=None, op0=ALU.is_gt), reads=[trif], writes=[trib])
        P.op("pool", lambda e: e.iota(eCi[:, :], pattern=[[C, NE]], base=1, channel_multiplier=0), writes=[eCi])
        P.op("dve", lambda e: e.tensor_copy(out=eC1[:, :], in_=eCi[:, :]), reads=[eCi], writes=[eC1])

        def v3(buf):
            return buf[:, :].rearrange("p (g e) -> p g e", g=8)

        def b3(buf8):
            return buf8[:, :].unsqueeze(2).to_broadcast([128, 8, 8])

        cnt = 0
        for tt in range(NT5):
            b, ti = divmod(tt, T5B)
            if ti == 0:
                bcast_load("sp", m2B, mod_d[b:b + 1, 4 * D:5 * D], reads=[B_mod])
                bcast_load("sp", hn, rvec_d[0:1, R_GFFN:R_GFFN + D])
                P.op("dve", lambda e: e.scalar_tensor_tensor(out=m2B[:, :], in0=m2B[:, :], scalar=1.0, in1=hn[:, :],
                                                             op0=ALU.add, op1=ALU.mult), reads=[m2B, hn], writes=[m2B])
                bcast_load("sp", shfB, mod_d[b:b + 1, 3 * D:4 * D], reads=[B_mod])
                bcast_load("sp", gfB, mod_d[b:b + 1, 5 * D:6 * D], reads=[B_mod])
            for s in range(4):
                tile = tt * 4 + s
                i2 = s % 2
                xt = x1s[s]
                r0 = tile * 128
                P.dma("sp", lambda e, xt=xt, r0=r0: e.dma_start(out=xt[:, :], in_=x1_d[r0:r0 + 128, :]), reads=[B_x1], writes=[xt])
                h2 = h2tm[tile % 5]
                rms_front((junk, ssq[i2], rstd[i2], hn), xt, m2B, shfB, h2)
                transposes(h2, pT, h2T, s, ("act", "dve"))

                def mmr(e, s=s):
                    for kc in range(KC):
                        ins = e.matmul(plog[:, 0:NE], lhsT=h2T[:, kc, s * 128:(s + 1) * 128], rhs=wrb[:, kc, :],
                                       start=(kc == 0), stop=(kc == KC - 1))
                    return ins
                P.op("pe", mmr, reads=[h2T, wrb], writes=[plog])
                sc, bi, eq, t_, mkb, masked, sel, ws, lt, g2, selv, pf, jk = (R[n] for n in ("sc", "bi", "eq", "t", "mk", "masked", "sel", "ws", "lt", "g2", "selv", "pf", "jk"))
                m1, m2, gs, g8, gm, off, m8, p8 = (R8[n] for n in ("m1", "m2", "gs", "g8", "gm", "off", "m8", "p8"))
                P.op("act", lambda e: e.activation(out=sc[:, :], in_=plog[:, 0:NE], func=AF.Sigmoid), reads=[plog], writes=[sc])
                P.op("dve", lambda e: e.tensor_tensor(out=bi[:, :], in0=sc[:, :], in1=rbB[:, :], op=ALU.add), reads=[sc, rbB], writes=[bi])
                P.op("dve", lambda e: e.tensor_reduce(out=m1[:, :], in_=v3(bi), axis=AX.X, op=ALU.max), reads=[bi], writes=[m1])
                P.op("dve", lambda e: e.tensor_tensor(out=v3(eq), in0=v3(bi), in1=b3(m1), op=ALU.is_equal), reads=[bi, m1], writes=[eq])
                P.op("dve", lambda e: e.scalar_tensor_tensor(out=t_[:, :], in0=eq[:, :], scalar=-4.0, in1=bi[:, :], op0=ALU.mult, op1=ALU.add),
                     reads=[eq, bi], writes=[t_])
                P.op("dve", lambda e: e.tensor_reduce(out=m2[:, :], in_=v3(t_), axis=AX.X, op=ALU.max), reads=[t_], writes=[m2])
                P.op("dve", lambda e: e.tensor_tensor(out=gs[:, :], in0=m1[:, :], in1=m2[:, :], op=ALU.add), reads=[m1, m2], writes=[gs])
                P.op("dve", lambda e: e.max(out=g8[:, :], in_=gs[:, :]), reads=[gs], writes=[g8])
                P.op("dve", lambda e: e.tensor_scalar(out=gm[:, :], in0=gs[:, :], scalar1=g8[:, 3:4], scalar2=None, op0=ALU.is_ge),
                     reads=[gs, g8], writes=[gm])
                P.op("dve", lambda e: e.tensor_tensor(out=v3(mkb), in0=v3(bi), in1=b3(gm), op=ALU.mult), reads=[bi, gm], writes=[mkb])
                P.op("dve", lambda e: e.tensor_scalar(out=off[:, :], in0=gm[:, :], scalar1=4.0, scalar2=-4.0, op0=ALU.mult, op1=ALU.add),
                     reads=[gm], writes=[off])
                P.op("dve", lambda e: e.tensor_tensor(out=v3(masked), in0=v3(mkb), in1=b3(off), op=ALU.add), reads=[mkb, off], writes=[masked])
                P.op("dve", lambda e: e.max(out=m8[:, :], in_=masked[:, :]), reads=[masked], writes=[m8])
                P.op("dve", lambda e: e.tensor_scalar(out=sel[:, :], in0=masked[:, :], scalar1=m8[:, 7:8], scalar2=None, op0=ALU.is_ge),
                     reads=[masked, m8], writes=[sel])
                P.op("dve", lambda e, tile=tile: e.tensor_copy(out=selall[:, tile, :], in_=sel[:, :]), reads=[sel], writes=[selall])
                P.op("dve", lambda e: e.memset(ssum[:, :], 0.0), reads=[ssum], writes=[ssum])
                P.op("dve", lambda e: e.scalar_tensor_tensor(out=ws[:, :], in0=sc[:, :], scalar=1.0, in1=sel[:, :], op0=ALU.mult, op1=ALU.mult,
                                                             accum_out=ssum[:, 0:1]), reads=[sc, sel, ssum], writes=[ws, ssum])
                P.op("dve", lambda e: e.reciprocal(out=ssum[:, :], in_=ssum[:, :]), reads=[ssum], writes=[ssum])
                P.op("dve", lambda e: e.tensor_scalar(out=ssum[:, :], in0=ssum[:, :], scalar1=2.5, scalar2=None, op0=ALU.mult),
                     reads=[ssum], writes=[ssum])

                def mrk(e, tile=tile):
                    for jt in range(tile):
                        e.matmul(prk[:, 0:NE], lhsT=onesb[:, :], rhs=selall[:, jt, :], start=(jt == 0), stop=False)
                    return e.matmul(prk[:, 0:NE], lhsT=trib[:, :], rhs=selall[:, tile, :], start=(tile == 0), stop=True)
                P.op("pe", mrk, reads=[onesb, trib, selall], writes=[prk])
                P.op("dve", lambda e: e.tensor_scalar(out=lt[:, :], in0=prk[:, 0:NE], scalar1=float(C), scalar2=None, op0=ALU.is_lt),
                     reads=[prk], writes=[lt])
                P.op("dve", lambda e: e.scalar_tensor_tensor(out=g2[:, :], in0=ws[:, :], scalar=ssum[:, 0:1], in1=lt[:, :], op0=ALU.mult, op1=ALU.mult),
                     reads=[ws, ssum, lt], writes=[g2])
                P.op("dve", lambda e: e.tensor_tensor(out=selv[:, :], in0=sel[:, :], in1=lt[:, :], op=ALU.mult), reads=[sel, lt], writes=[selv])
                P.op("dve", lambda e: e.tensor_tensor(out=pf[:, :], in0=prk[:, 0:NE], in1=eC1[:, :], op=ALU.add), reads=[prk, eC1], writes=[pf])
                P.op("dve", lambda e: e.tensor_tensor(out=pf[:, :], in0=pf[:, :], in1=selv[:, :], op=ALU.mult), reads=[pf, selv], writes=[pf])
                P.op("dve", lambda e: e.tensor_scalar(out=pf[:, :], in0=pf[:, :], scalar1=-1.0, scalar2=None, op0=ALU.add), reads=[pf], writes=[pf])
                P.op("dve", lambda e: e.max(out=p8[:, :], in_=pf[:, :]), reads=[pf], writes=[p8])
                P.op("dve", lambda e: e.tensor_scalar(out=R8["m2"][:, :], in0=p8[:, :], scalar1=0.0, scalar2=None, op0=ALU.is_lt), reads=[p8], writes=[R8["m2"]])
                P.op("dve", lambda e: e.scalar_tensor_tensor(out=R8["m1"][:, :], in0=R8["m2"][:, :], scalar=600000.0, in1=p8[:, :], op0=ALU.mult, op1=ALU.add),
                     reads=[R8["m2"], p8], writes=[R8["m1"]])
                P.op("dve", lambda e, tile=tile: e.tensor_copy(out=posall[:, tile * 8:(tile + 1) * 8], in_=R8["m1"][:, :]), reads=[R8["m1"]], writes=[posall])
                for k in range(8):
                    P.op("dve", lambda e, tile=tile, k=k: e.scalar_tensor_tensor(
                        out=jk[:, :], in0=pf[:, :], scalar=p8[:, k:k + 1], in1=g2[:, :], op0=ALU.is_equal, op1=ALU.mult,
                        accum_out=wall[:, tile * 8 + k:tile * 8 + k + 1]), reads=[pf, p8, g2, wall], writes=[jk, wall])
                for k in range(8):
                    P.dma("pool", lambda e, h2=h2, tile=tile, k=k: e.indirect_dma_start(
                        out=xb_d[:, :], out_offset=bass.IndirectOffsetOnAxis(ap=posall[:, tile * 8 + k:tile * 8 + k + 1], axis=0),
                        in_=h2[:, :], in_offset=None, bounds_check=get_bc(e, 2), oob_is_err=False),
                        reads=[h2, posall, B_xb], writes=[B_xb])
                if dbg:
                    P.dma("sp", lambda e, r0=r0, tile=tile: e.dma_start(out=dbg_d[r0:r0 + 128, 8:16], in_=wall[:, tile * 8:(tile + 1) * 8]),
                          reads=[wall], writes=[B_out])
            for fc in range(4):
                pg = pgu[(2 * fc) % 4]
                pu = pgu[(2 * fc + 1) % 4]

                def mmg(e, pg=pg, fc=fc, w=wsg):
                    for kc in range(KC):
                        ins = e.matmul(pg[:, :], lhsT=w[:, kc, fc * 128:(fc + 1) * 128], rhs=h2T[:, kc, :], start=(kc == 0), stop=(kc == KC - 1))
                    return ins

                def mmu(e, pu=pu, fc=fc, w=wsu):
                    for kc in range(KC):
                        ins = e.matmul(pu[:, :], lhsT=w[:, kc, fc * 128:(fc + 1) * 128], rhs=h2T[:, kc, :], start=(kc == 0), stop=(kc == KC - 1))
                    return ins
                P.op("pe", mmg, reads=[wsg, h2T], writes=[pg])
                P.op("pe", mmu, reads=[wsu, h2T], writes=[pu])
                sgb = sgs[fc % 2]
                P.op("act", lambda e, pg=pg, sgb=sgb: e.activation(out=sgb[:, :], in_=pg[:, :], func=AF.Silu), reads=[pg], writes=[sgb])
                P.op("dve", lambda e, pu=pu, sgb=sgb, fc=fc: e.tensor_tensor(out=AT[:, fc, :], in0=pu[:, :], in1=sgb[:, :], op=ALU.mult),
                     reads=[pu, sgb], writes=[AT])
            for s in range(4):
                xt = x1s[s]
                r0 = (tt * 4 + s) * 128
                for n in range(4):
                    pb = pgu[cnt % 4]
                    tm = tmpo[cnt % 2]
                    cnt += 1

                    def mmd(e, pb=pb, s=s, n=n):
                        for fc in range(4):
                            ins = e.matmul(pb[:, :], lhsT=AT[:, fc, s * 128:(s + 1) * 128], rhs=wsd[:, fc, n * 512:(n + 1) * 512],
                                           start=(fc == 0), stop=(fc == 3))
                        return ins
                    P.op("pe", mmd, reads=[AT, wsd], writes=[pb])
                    P.op("dve", lambda e, pb=pb, tm=tm, n=n: e.tensor_tensor(out=tm[:, :], in0=pb[:, :], in1=gfB[:, n * 512:(n + 1) * 512], op=ALU.mult),
                         reads=[pb, gfB], writes=[tm])
                    P.op("pool", lambda e, tm=tm, xt=xt, n=n: e.tensor_tensor(out=xt[:, n * 512:(n + 1) * 512], in0=tm[:, :],
                                                                               in1=xt[:, n * 512:(n + 1) * 512], op=ALU.add),
                         reads=[tm, xt], writes=[xt])
                P.dma("sp", lambda e, xt=xt, r0=r0: e.dma_start(out=x2_d[r0:r0 + 128, :], in_=xt[:, :]), reads=[xt], writes=[B_x2])
        P.flush()

    if phases < 4:
        P.barrier()
        P.flush(final=True)
        top.close()
        return nc

    P.new_phase_sems()
    with ExitStack() as es:
        P.barrier()
        wg = [mk(es, f"wg{i}", [128, KC, FE], BF16) for i in range(2)]
        wu = [mk(es, f"wu{i}", [128, KC, FE], BF16) for i in range(2)]
        wd = [mk(es, f"wd{i}", [128, 4, D], BF16) for i in range(2)]
        xrow = [mk(es, f"xrow{i}", [128, D], BF16) for i in range(4)]
        XT = [mk(es, f"XT{i}", [128, KC, 512], BF16) for i in range(2)]
        ATe = [mk(es, f"ATe{i}", [128, 4, 512], BF16) for i in range(2)]
        sge = [mk(es, f"sge{i}", [128, 512], F32) for i in range(2)]
        yst = [mk(es, f"yst{i}", [128, D], BF16) for i in range(3)]
        pT = [mkp(es, f"pT3{i}", [128, 1024], BF16) for i in range(2)]
        pgu = [mkp(es, f"pgu3{i}", [128, 512], F32) for i in range(4)]
        py = [mkp(es, f"py3{i}", [128, 512], F32) for i in range(2)]
        gcnt = 0
        rowc = 0
        ycnt = 0
        yscnt = 0

        def load_w(e_):
            i = e_ % 2
            P.dma("pool", lambda e: e.dma_start(out=wg[i][:, :, :], in_=wg_d[e_]), writes=[wg[i]])
            P.dma("pool", lambda e: e.dma_start(out=wu[i][:, :, :], in_=wu_d[e_]), writes=[wu[i]])
            P.dma("pool", lambda e: e.dma_start(out=wd[i][:, :, :], in_=wd_d[e_]), writes=[wd[i]])

        load_w(0)
        for ex in range(NE):
            if ex + 1 < NE:
                load_w(ex + 1)
            wi = ex % 2
            for (g0, gn) in groups:
                xt_ = XT[gcnt % 2]
                at_ = ATe[gcnt % 2]
                gcnt += 1
                ns = gn // 128
                for s in range(ns):
                    xr_ = xrow[rowc % 4]
                    rowc += 1
                    r0 = ex * C + g0 + s * 128
                    P.dma("sp", lambda e, xr_=xr_, r0=r0: e.dma_start(out=xr_[:, :], in_=xb_d[r0:r0 + 128, :]), reads=[B_xb], writes=[xr_])
                    transposes(xr_, pT, xt_, s, ("act", "dve"))
                for fc in range(4):
                    pg = pgu[(2 * fc) % 4]
                    pu = pgu[(2 * fc + 1) % 4]

                    def mmg(e, pg=pg, fc=fc, w=wg[wi], xt_=xt_, gn=gn):
                        for kc in range(KC):
                            ins = e.matmul(pg[:, 0:gn], lhsT=w[:, kc, fc * 128:(fc + 1) * 128], rhs=xt_[:, kc, 0:gn], start=(kc == 0), stop=(kc == KC - 1))
                        return ins

                    def mmu(e, pu=pu, fc=fc, w=wu[wi], xt_=xt_, gn=gn):
                        for kc in range(KC):
                            ins = e.matmul(pu[:, 0:gn], lhsT=w[:, kc, fc * 128:(fc + 1) * 128], rhs=xt_[:, kc, 0:gn], start=(kc == 0), stop=(kc == KC - 1))
                        return ins
                    P.op("pe", mmg, reads=[wg[wi], xt_], writes=[pg])
                    P.op("pe", mmu, reads=[wu[wi], xt_], writes=[pu])
                    sgb = sge[fc % 2]
                    P.op("act", lambda e, pg=pg, sgb=sgb, gn=gn: e.activation(out=sgb[:, 0:gn], in_=pg[:, 0:gn], func=AF.Silu), reads=[pg], writes=[sgb])
                    P.op("dve", lambda e, pu=pu, sgb=sgb, fc=fc, at_=at_, gn=gn: e.tensor_tensor(out=at_[:, fc, 0:gn], in0=pu[:, 0:gn], in1=sgb[:, 0:gn], op=ALU.mult),
                         reads=[pu, sgb], writes=[at_])
                for s in range(ns):
                    ys = yst[yscnt % 3]
                    yscnt += 1
                    r0 = ex * C + g0 + s * 128
                    for n in range(4):
                        pb = py[ycnt % 2]
                        ycnt += 1

                        def mmd(e, pb=pb, s=s, n=n, at_=at_, w=wd[wi]):
                            for fc in range(4):
                                ins = e.matmul(pb[:, :], lhsT=at_[:, fc, s * 128:(s + 1) * 128], rhs=w[:, fc, n * 512:(n + 1) * 512],
                                               start=(fc == 0), stop=(fc == 3))
                            return ins
                        P.op("pe", mmd, reads=[at_, wd[wi]], writes=[pb])
                        if n % 2 == 0:
                            P.op("act", lambda e, pb=pb, ys=ys, n=n: e.copy(out=ys[:, n * 512:(n + 1) * 512], in_=pb[:, :]), reads=[pb], writes=[ys])
                        else:
                            P.op("dve", lambda e, pb=pb, ys=ys, n=n: e.tensor_copy(out=ys[:, n * 512:(n + 1) * 512], in_=pb[:, :]), reads=[pb], writes=[ys])
                    P.dma("sp", lambda e, ys=ys, r0=r0: e.dma_start(out=yb_d[r0:r0 + 128, :], in_=ys[:, :]), reads=[ys], writes=[B_yb])
        P.flush()

    if phases < 5:
        P.barrier()
        P.flush(final=True)
        top.close()
        return nc

    P.new_phase_sems()
    with ExitStack() as es:
        P.barrier()
        NG = 16
        gsl = [mk(es, f"gsl{i}", [128, D], BF16) for i in range(NG)]
        gfB = mk(es, "gfB4", [128, D], F32)
        gfinB = mk(es, "gfinB", [128, D], F32)
        x2s = [mk(es, f"x2s{i}", [128, D], F32) for i in range(2)]
        acc = [mk(es, f"acc4{i}", [128, D], F32) for i in range(2)]
        junk = mk(es, "junk4", [128, D], BF16)
        ssq = [mk(es, f"ssq4{i}", [128, 1], F32) for i in range(2)]
        rstd = [mk(es, f"rstd4{i}", [128, 1], F32) for i in range(2)]
        for i in range(NG):
            P.op("pool" if i % 2 else "dve", lambda e, i=i: e.memset(gsl[i][:, :], 0.0), writes=[gsl[i]])
        bcast_load("sp", gfinB, rvec_d[0:1, R_GFIN:R_GFIN + D])
        gc = 0
        gather_evs = []
        for tile in range(NT):
            b = (tile * 128) // S
            if (tile * 128) % S == 0:
                bcast_load("sp", gfB, mod_d[b:b + 1, 5 * D:6 * D], reads=[B_mod])
            i2 = tile % 2
            r0 = tile * 128
            xt = x2s[i2]
            P.dma("sp", lambda e, xt=xt, r0=r0: e.dma_start(out=xt[:, :], in_=x2_d[r0:r0 + 128, :]), reads=[B_x2], writes=[xt])
            a = acc[i2]
            for k in range(8):
                gt = gsl[gc % NG]
                gc += 1
                if True:
                  P.dma("pool", lambda e, gt=gt, tile=tile, k=k: e.indirect_dma_start(
                    out=gt[:, :], out_offset=None, in_=yb_d[:, :],
                    in_offset=bass.IndirectOffsetOnAxis(ap=posall[:, tile * 8 + k:tile * 8 + k + 1], axis=0),
                    bounds_check=get_bc(e, 4), oob_is_err=False), reads=[B_yb, posall, gt], writes=[gt],
                    extra=[gather_evs[-GSER]] if len(gather_evs) >= GSER else [])
                  gather_evs.append(gt.lw)
                wcol = wall[:, tile * 8 + k:tile * 8 + k + 1]
                if k == 0:
                    P.op("dve", lambda e, gt=gt, a=a, wcol=wcol: e.tensor_scalar(out=a[:, :], in0=gt[:, :], scalar1=wcol, scalar2=None, op0=ALU.mult),
                         reads=[gt, wall], writes=[a])
                else:
                    P.op("dve", lambda e, gt=gt, a=a, wcol=wcol: e.scalar_tensor_tensor(out=a[:, :], in0=gt[:, :], scalar=wcol, in1=a[:, :],
                                                                                         op0=ALU.mult, op1=ALU.add), reads=[gt, wall, a], writes=[a])
            P.op("pool", lambda e, a=a: e.tensor_tensor(out=a[:, :], in0=a[:, :], in1=gfB[:, :], op=ALU.mult), reads=[a, gfB], writes=[a])
            P.op("pool", lambda e, a=a, xt=xt: e.tensor_tensor(out=xt[:, :], in0=a[:, :], in1=xt[:, :], op=ALU.add), reads=[a, xt], writes=[xt])
            P.op("act", lambda e, xt=xt, i2=i2: e.activation(out=junk[:, :], in_=xt[:, :], func=AF.Square, accum_out=ssq[i2][:, 0:1]),
                 reads=[xt], writes=[junk, ssq[i2]])
            P.op("act", lambda e, i2=i2: e.activation(out=rstd[i2][:, 0:1], in_=ssq[i2][:, 0:1], func=AF.Sqrt, bias=epsT[:, 0:1], scale=1.0 / D),
                 reads=[ssq[i2], epsT], writes=[rstd[i2]])
            P.op("dve", lambda e, i2=i2: e.reciprocal(out=rstd[i2][:, 0:1], in_=rstd[i2][:, 0:1]), reads=[rstd[i2]], writes=[rstd[i2]])
            P.op("dve", lambda e, xt=xt, a=a, i2=i2: e.scalar_tensor_tensor(out=a[:, :], in0=xt[:, :], scalar=rstd[i2][:, 0:1], in1=gfinB[:, :],
                                                                            op0=ALU.mult, op1=ALU.mult), reads=[xt, rstd[i2], gfinB], writes=[a])
            P.dma("sp", lambda e, a=a, r0=r0: e.dma_start(out=out_d[r0:r0 + 128, :], in_=a[:, :]), reads=[a], writes=[B_out])
        P.flush(final=True)
    top.close()
    return nc


def prep_shared(inp):
    f32 = np.float32
    sh = {}
    sh["w_ada"] = np.ascontiguousarray(inp["w_ada"][0].reshape(KC, 128, 6 * D).transpose(1, 0, 2))
    W = inp["w_in"][0]
    sh["w_in"] = np.ascontiguousarray(W.reshape(KC, 128, 5, 8, 128).transpose(3, 1, 0, 2, 4).reshape(8, 128, KC, 640))
    sh["w_out"] = np.ascontiguousarray(inp["w_out"][0].reshape(KC, 128, D).transpose(1, 0, 2))
    sh["w_router"] = np.ascontiguousarray(inp["w_router"][0].reshape(KC, 128, NE).transpose(1, 0, 2))
    sh["ws_gate"] = np.ascontiguousarray(inp["w_shared_gate"][0].reshape(KC, 128, FE).transpose(1, 0, 2))
    sh["ws_up"] = np.ascontiguousarray(inp["w_shared_up"][0].reshape(KC, 128, FE).transpose(1, 0, 2))
    sh["ws_down"] = np.ascontiguousarray(inp["w_shared_down"][0].reshape(4, 128, D).transpose(1, 0, 2))
    sh["w_gate"] = np.ascontiguousarray(inp["w_gate"][0].reshape(NE, KC, 128, FE).transpose(0, 2, 1, 3))
    sh["w_up"] = np.ascontiguousarray(inp["w_up"][0].reshape(NE, KC, 128, FE).transpose(0, 2, 1, 3))
    sh["w_down"] = np.ascontiguousarray(inp["w_down"][0].reshape(NE, 4, 128, D).transpose(0, 2, 1, 3))
    fv = np.zeros((128, NF), f32)
    fv[:, FA_W:FA_W + 24] = inp["conv_a_w"][0].reshape(3, 8, 128).transpose(2, 1, 0).reshape(128, 24)
    fv[:, FB_W:FB_W + 248] = inp["conv_b_w"][0].reshape(31, 8, 128).transpose(2, 1, 0).reshape(128, 248)
    for off, key in ((FB_B, "conv_b_b"), (FLN_G, "ln_b_g"), (FLN_B, "ln_b_b"), (FHA, "head_norm_a_g"), (FHB, "head_norm_b_g")):
        fv[:, off:off + 8] = inp[key][0].reshape(8, 128).T
    sh["fvec"] = fv
    rv = np.zeros((1, NR), f32)
    rv[0, R_GMIX:R_GMIX + D] = inp["norm_mix_g"][0]
    rv[0, R_GFFN:R_GFFN + D] = inp["norm_ffn_g"][0]
    rv[0, R_GFIN:R_GFIN + D] = inp["norm_final_g"]
    rv[0, R_RB:R_RB + NE] = inp["router_bias"][0]
    rv[0, R_BADA:R_BADA + 6 * D] = inp["b_ada"][0]
    sh["rvec"] = rv
    return sh


def core_inputs(sh, x_core, c_core):
    NB = c_core.shape[0]
    m = dict(sh)
    m["x"] = np.ascontiguousarray(x_core.reshape(-1, D))
    m["cT"] = np.ascontiguousarray(c_core.reshape(NB, KC, 128).transpose(2, 1, 0))
    return m


CAP = 1024
GSER = 1


def kernel(**inputs):
    inp = {k: np.asarray(v) for k, v in inputs.items()}
    x = inp["x"]
    B, S, _ = x.shape
    NB = B // NCORES
    sh = prep_shared(inp)
    in_maps = [core_inputs(sh, x[i * NB:(i + 1) * NB], inp["c"][i * NB:(i + 1) * NB]) for i in range(NCORES)]
    nc = build(NB, S, CAP)
    res = run_bass_kernel_spmd(nc, in_maps, core_ids=list(range(NCORES)))
    out = np.concatenate([r["out"].reshape(NB, S, D) for r in res.results], axis=0)
    return out.astype(np.float32)
```

```python
import os
import numpy as np
from contextlib import ExitStack
import concourse.bass as bass
import concourse.mybir as mybir
from concourse.bass_utils import run_bass_kernel_spmd

F32 = mybir.dt.float32
BF16 = mybir.dt.bfloat16
I32 = mybir.dt.int32
ALU = mybir.AluOpType
AF = mybir.ActivationFunctionType
AX = mybir.AxisListType

D = 2048
KC = 16
NE = 64
FE = 512
EPS = 1e-6
NCORES = 8

ENGS = ("pe", "act", "dve", "pool", "sp")


class Buf:
    __slots__ = ("name", "t", "lw", "rd")

    def __init__(self, name, t=None):
        self.name = name
        self.t = t
        self.lw = None
        self.rd = []

    def __getitem__(self, k):
        return self.t[k]


class Prog:
    def __init__(self, nc, n_dma_sems=12):
        self.nc = nc
        self.es = ExitStack()
        self.eng = {"pe": nc.tensor, "act": nc.scalar, "dve": nc.vector,
                    "pool": nc.gpsimd, "sp": nc.sync}
        self.ops = {e: [] for e in ENGS}
        self.waited = {e: {} for e in ENGS}
        self.psem = {}
        self.pcnt = {}
        self.n_dma_sems = n_dma_sems
        self.dsem = {}
        self.dcnt = {}
        self.dlast = {}
        self.drr = {}
        self.last_ev = {e: None for e in ENGS}
        self.gen = 0
        self.cond = None
        self.cregs = {}
        self.cnt_ap = None
        self.cnt_ev = None
        for q in ("sp", "pool", "act"):
            self.dsem[q] = [self._sem(f"d_{q}{i}") for i in range(n_dma_sems)]
            self.dcnt[q] = [0] * n_dma_sems
            self.dlast[q] = [None] * n_dma_sems
            self.drr[q] = 0
        self.new_phase_sems()

    def cond_reg(self, e, engine, idx):
        st = self.cregs.get(e)
        if st is None:
            st = {"reg": engine.alloc_register(f"cnt_{e}"), "idx": None}
            self.cregs[e] = st
        if st["idx"] != idx:
            engine.wait_ge(self.cnt_ev[0], self.cnt_ev[1])
            engine.reg_load(st["reg"], self.cnt_ap(idx))
            st["idx"] = idx
        return st["reg"]

    def _sem(self, name):
        return self.es.enter_context(self.nc.semaphore(name))

    def new_phase_sems(self):
        self.gen += 1
        for e in ("pe", "act", "dve", "pool"):
            self.psem[e] = self._sem(f"p{self.gen}_{e}")
            self.pcnt[e] = 0

    def _need(self, e, evs):
        w = self.waited[e]
        best = {}
        for ev in evs:
            if ev is None:
                continue
            s, v = ev
            if w.get(id(s), 0) >= v:
                continue
            if id(s) not in best or best[id(s)][1] < v:
                best[id(s)] = (s, v)
        out = []
        for k, (s, v) in best.items():
            w[k] = v
            out.append((s, v))
        return out

    @staticmethod
    def _deps(reads, writes):
        evs = []
        for b in reads:
            evs.append(b.lw)
        for b in writes:
            evs.append(b.lw)
            evs.extend(b.rd)
        return evs

    @staticmethod
    def _commit(ev, reads, writes):
        for b in reads:
            b.rd.append(ev)
        for b in writes:
            b.lw = ev
            b.rd = []

    def begin_cond(self, idx, thr):
        self.cond = (idx, thr)
        self.cond_snap = {e: dict(self.waited[e]) for e in ENGS}

    def end_cond(self):
        self.cond = None
        for e in ENGS:
            self.waited[e] = self.cond_snap[e]

    def op(self, e, fn, reads=(), writes=(), extra=()):
        evs = self._deps(reads, writes) + list(extra)
        waits = self._need(e, evs)
        self.pcnt[e] += 1
        ev = (self.psem[e], self.pcnt[e])
        self.ops[e].append((waits, fn, ev, 1, self.cond, None))
        self._commit(ev, reads, writes)
        self.last_ev[e] = ev
        return ev

    def dma(self, q, fn, reads=(), writes=(), extra=()):
        i = self.drr[q]
        self.drr[q] = (i + 1) % self.n_dma_sems
        evs = self._deps(reads, writes) + list(extra) + [self.dlast[q][i]]
        waits = self._need(q, evs)
        prev = self.dcnt[q][i]
        self.dcnt[q][i] += 16
        ev = (self.dsem[q][i], self.dcnt[q][i])
        self.dlast[q][i] = ev
        self.ops[q].append((waits, fn, ev, 16, self.cond, prev))
        self._commit(ev, reads, writes)
        return ev

    def barrier(self):
        evs = [self.last_ev[e] for e in ("pe", "act", "dve", "pool")]
        for q in ("sp", "pool", "act"):
            evs.extend(self.dlast[q])
        for e in ENGS:
            waits = self._need(e, evs)
            if waits:
                self.ops[e].append((waits, None, None, 0, None, None))

    def flush(self, final=False):
        if final:
            self.barrier()
        ops = self.ops

        def emit_one(engine, o):
            waits, fn, ev, inc, _, _ = o
            for s, v in waits:
                engine.wait_ge(s, v)
            if fn is not None:
                ins = fn(engine)
                ins.then_inc(ev[0], inc)

        def emit(e):
            def body(engine):
                lst = ops[e]
                i = 0
                while i < len(lst):
                    c = lst[i][4]
                    if c is None:
                        emit_one(engine, lst[i])
                        i += 1
                        continue
                    j = i
                    while j < len(lst) and lst[j][4] == c:
                        j += 1
                    run = lst[i:j]
                    reg = self.cond_reg(e, engine, c[0])
                    with engine.If_lt(reg, c[1] + 1):
                        ninc = {}
                        for (_, fn, ev, inc, _, prev) in run:
                            if fn is None:
                                continue
                            if inc == 1:
                                if id(ev[0]) not in ninc:
                                    ninc[id(ev[0])] = [ev[0], 0, ev[1] - 1]
                                ninc[id(ev[0])][1] += 1
                            else:
                                if prev:
                                    engine.wait_ge(ev[0], prev)
                                engine.sem_inc(ev[0], 16)
                        for (sm, n, v0) in ninc.values():
                            if v0 > 0:
                                engine.wait_ge(sm, v0)
                            engine.sem_inc(sm, n)
                    with engine.Else():
                        for o in run:
                            emit_one(engine, o)
                    i = j
            return body

        with self.nc.Block() as block:
            block.tensor(emit("pe"))
            block.scalar(emit("act"))
            block.vector(emit("dve"))
            block.gpsimd(emit("pool"))
            block.sync(emit("sp"))
        self.ops = {e: [] for e in ENGS}


FA_W = 0
FB_W = 24
FB_B = 24 + 248
FLN_G = FB_B + 8
FLN_B = FLN_G + 8
FHA = FLN_B + 8
FHB = FHA + 8
NF = FHB + 8
R_GMIX = 0
R_GFFN = D
R_GFIN = 2 * D
R_RB = 3 * D
R_BADA = 3 * D + NE
NR = R_BADA + 6 * D


def build(NB, S, C, phases=5, dbg=False):
    TC = NB * S
    NT = TC // 128
    NT5 = TC // 512
    T5B = S // 512
    NSLOT = NE * C
    groups = [(0, C // 2), (C // 2, C // 4), (3 * C // 4, C // 4)]
    assert C // 2 <= 512 and (C // 4) % 128 == 0

    nc = bass.Bass("TRN2", target_bir_lowering=False)
    dt_in = lambda name, shape: nc.dram_tensor(name, shape, F32, kind="ExternalInput").ap()
    x_d = dt_in("x", [TC, D])
    cT_d = dt_in("cT", [128, KC, NB])
    wada_d = dt_in("w_ada", [128, KC, 6 * D])
    rvec_d = dt_in("rvec", [1, NR])
    fvec_d = dt_in("fvec", [128, NF])
    win_d = dt_in("w_in", [8, 128, KC, 640])
    wout_d = dt_in("w_out", [128, KC, D])
    wr_d = dt_in("w_router", [128, KC, NE])
    wsg_d = dt_in("ws_gate", [128, KC, FE])
    wsu_d = dt_in("ws_up", [128, KC, FE])
    wsd_d = dt_in("ws_down", [128, 4, D])
    wg_d = dt_in("w_gate", [NE, 128, KC, FE])
    wu_d = dt_in("w_up", [NE, 128, KC, FE])
    wd_d = dt_in("w_down", [NE, 128, 4, D])
    out_d = nc.dram_tensor("out", [TC, D], F32, kind="ExternalOutput").ap()
    sk = "ExternalOutput" if dbg else "Internal"
    mod_d = nc.dram_tensor("modd", [NB, 6 * D], F32, kind=sk).ap()
    yT_d = nc.dram_tensor("yTd", [NT5, 128, KC, 512], BF16, kind=sk).ap()
    x1_d = nc.dram_tensor("x1d", [TC, D], F32, kind=sk).ap()
    x2_d = nc.dram_tensor("x2d", [TC, D], F32, kind=sk).ap()
    xb_d = nc.dram_tensor("xbuf", [NSLOT, D], BF16, kind="Internal").ap()
    yb_d = nc.dram_tensor("ybuf", [NSLOT, D], BF16, kind="Internal").ap()
    if dbg:
        dbg_d = nc.dram_tensor("dbg", [TC, 16], F32, kind="ExternalOutput").ap()
    B_mod, B_yT, B_x1, B_x2, B_xb, B_yb, B_out = (Buf(n) for n in ("modd", "yTd", "x1d", "x2d", "xbuf", "ybuf", "outd"))

    P = Prog(nc)
    top = ExitStack()
    bc_reg = {}

    def get_bc(e, key):
        if key not in bc_reg:
            r_ = e.alloc_register(f"bc{key}")
            e.reg_mov(r_, NSLOT - 1)
            bc_reg[key] = r_
        return bc_reg[key]

    def mk(es, name, shape, dt):
        return Buf(name, es.enter_context(nc.sbuf_tensor("s_" + name, shape, dt)))

    def mkp(es, name, shape, dt):
        return Buf(name, es.enter_context(nc.psum_tensor("q_" + name, shape, dt)))

    posall = mk(top, "posall", [128, NT * 8], I32)
    wall = mk(top, "wall", [128, NT * 8], F32)
    ident = mk(top, "ident", [128, 128], BF16)
    epsT = mk(top, "epsT", [128, 1], F32)
    fvec = mk(top, "fvec", [128, NF], F32)
    cnts = mk(top, "cnts", [1, NE], I32)
    zt = mk(top, "zt", [128, 2048], BF16)

    with ExitStack() as es:
        identf = mk(es, "identf", [128, 128], F32)
        cT = mk(es, "cT", [128, KC, NB], F32)
        cact = mk(es, "cact", [128, KC, NB], F32)
        wa = [mk(es, f"wa{i}", [128, KC, 512], F32) for i in range(2)]
        modsb = mk(es, "modsb", [NB, 6 * D], F32)
        badaB = mk(es, "badaB", [NB, 6 * D], F32)
        pm = [mkp(es, f"pm{i}", [128, 512], F32) for i in range(2)]

        P.op("pool", lambda e: e.memset(identf[:, :], 0.0), writes=[identf])
        P.op("pool", lambda e: e.affine_select(out=identf[:, :], in_=identf[:, :], pattern=[[-1, 128]],
                                               compare_op=ALU.not_equal, fill=1.0, base=0, channel_multiplier=1),
             reads=[identf], writes=[identf])
        P.op("dve", lambda e: e.tensor_copy(out=ident[:, :], in_=identf[:, :]), reads=[identf], writes=[ident])
        P.op("dve", lambda e: e.memset(epsT[:, :], EPS), writes=[epsT])
        P.op("dve", lambda e: e.memset(wall[:, :], 0.0), writes=[wall])
        P.op("pool", lambda e: e.memset(zt[:, :], 0.0), writes=[zt])
        P.dma("sp", lambda e: e.dma_start(out=fvec[:, :], in_=fvec_d[:, :]), writes=[fvec])
        P.dma("sp", lambda e: e.dma_start(out=cT[:, :, :], in_=cT_d[:, :, :]), writes=[cT])
        P.dma("sp", lambda e: e.dma_start(out=badaB[:, :], in_=rvec_d[0:1, R_BADA:R_BADA + 6 * D].partition_broadcast(NB)),
              writes=[badaB])
        P.op("act", lambda e: e.activation(out=cact[:, :, :], in_=cT[:, :, :], func=AF.Silu), reads=[cT], writes=[cact])
        zf_list = list(range(NSLOT // 128))
        for nb in range(24):
            w = wa[nb % 2]
            P.dma("sp", lambda e, nb=nb, w=w: e.dma_start(out=w[:, :, :], in_=wada_d[:, :, nb * 512:(nb + 1) * 512]), writes=[w])
            pp = pm[nb % 2]

            def mm(e, w=w, pp=pp):
                for kc in range(KC):
                    ins = e.matmul(pp[0:NB, :], lhsT=cact[:, kc, :], rhs=w[:, kc, :], start=(kc == 0), stop=(kc == KC - 1))
                return ins
            P.op("pe", mm, reads=[cact, w], writes=[pp])
            P.op("dve", lambda e, nb=nb, pp=pp: e.tensor_tensor(out=modsb[:, nb * 512:(nb + 1) * 512], in0=pp[0:NB, :],
                                                                 in1=badaB[:, nb * 512:(nb + 1) * 512], op=ALU.add),
                 reads=[pp, badaB], writes=[modsb])
        P.dma("sp", lambda e: e.dma_start(out=mod_d[:, :], in_=modsb[:, :]), reads=[modsb], writes=[B_mod])
        P.flush()

    def bcast_load(q, dst, src_ap, nparts=128, reads=()):
        return P.dma(q, lambda e: e.dma_start(out=dst[:, :], in_=src_ap.partition_broadcast(nparts)), reads=list(reads), writes=[dst])

    def rms_front(es_bufs, xt, m_scale, m_shift, hout):
        junk, ssq, rstd, hn = es_bufs
        P.op("act", lambda e: e.activation(out=junk[:, :], in_=xt[:, :], func=AF.Square, accum_out=ssq[:, 0:1]),
             reads=[xt], writes=[junk, ssq])
        P.op("act", lambda e: e.activation(out=rstd[:, 0:1], in_=ssq[:, 0:1], func=AF.Sqrt, bias=epsT[:, 0:1], scale=1.0 / D),
             reads=[ssq, epsT], writes=[rstd])
        P.op("dve", lambda e: e.reciprocal(out=rstd[:, 0:1], in_=rstd[:, 0:1]), reads=[rstd], writes=[rstd])
        P.op("dve", lambda e: e.scalar_tensor_tensor(out=hn[:, :], in0=xt[:, :], scalar=rstd[:, 0:1], in1=m_scale[:, :],
                                                     op0=ALU.mult, op1=ALU.mult), reads=[xt, rstd, m_scale], writes=[hn])
        P.op("pool", lambda e: e.tensor_tensor(out=hout[:, :], in0=hn[:, :], in1=m_shift[:, :], op=ALU.add),
             reads=[hn, m_shift], writes=[hout])

    def transposes(src, pT, dstT, s, evac_engs):
        for h in range(2):
            pb = pT[h]

            def tr(e, h=h, pb=pb):
                for k in range(8):
                    kc = h * 8 + k
                    ins = e.transpose(out=pb[:, k * 128:(k + 1) * 128], in_=src[:, kc * 128:(kc + 1) * 128], identity=ident[:, :])
                return ins
            P.op("pe", tr, reads=[src, ident], writes=[pb])
            en = evac_engs[h]
            o_ap = dstT[:, h * 8:(h + 1) * 8, s * 128:(s + 1) * 128]
            i_ap = pb[:, :].rearrange("p (k t) -> p k t", k=8)
            if en == "act":
                P.op("act", lambda e, o_ap=o_ap, i_ap=i_ap: e.copy(out=o_ap, in_=i_ap), reads=[pb], writes=[dstT])
            else:
                P.op(en, lambda e, o_ap=o_ap, i_ap=i_ap: e.tensor_copy(out=o_ap, in_=i_ap), reads=[pb], writes=[dstT])

    if phases < 1:
        P.flush(final=True)
        top.close()
        return nc

    P.new_phase_sems()
    with ExitStack() as es:
        P.barrier()
        m1B = mk(es, "m1B", [128, D], F32)
        shmB = mk(es, "shmB", [128, D], F32)
        gmixB = mk(es, "gmixB", [128, D], F32)
        xs = [mk(es, f"xs{i}", [128, D], F32) for i in range(2)]
        hn = mk(es, "hn", [128, D], F32)
        junk = mk(es, "junk", [128, D], BF16)
        htm = [mk(es, f"htm{i}", [128, D], BF16) for i in range(2)]
        ssq = [mk(es, f"ssq{i}", [128, 1], F32) for i in range(2)]
        rstd = [mk(es, f"rstd{i}", [128, 1], F32) for i in range(2)]
        hT = mk(es, "hT", [128, KC, 512], BF16)
        win = [mk(es, f"win{i}", [128, KC, 640], BF16) for i in range(2)]
        yT = mk(es, "yT", [128, KC, 512], BF16)
        uhist = mk(es, "uhist", [128, 8, 2], F32)
        ghist = mk(es, "ghist", [128, 8, 30], F32)
        uw = [mk(es, f"uw{i}", [128, 514], F32) for i in range(2)]
        gw = [mk(es, f"gw{i}", [128, 542], F32) for i in range(2)]
        zb = [mk(es, f"zb{j}", [128, 512], F32) for j in range(8)]
        xa_s = [mk(es, f"xa_s{i}", [128, 512], F32) for i in range(2)]
        acc_a = [mk(es, f"acc_a{i}", [128, 512], F32) for i in range(2)]
        ya = [mk(es, f"ya{i}", [128, 512], F32) for i in range(2)]
        sq = [mk(es, f"sq{i}", [128, 512], F32) for i in range(2)]
        rs = [mk(es, f"rs{i}", [128, 512], F32) for i in range(2)]
        sg = [mk(es, f"sg{i}", [128, 512], F32) for i in range(2)]
        mean = mk(es, "mean", [128, 512], F32)
        msq = mk(es, "msq", [128, 512], F32)
        lrstd = mk(es, "lrstd", [128, 512], F32)
        t1 = [mk(es, f"t1{i}", [128, 512], F32) for i in range(2)]
        zz = [mk(es, f"zz{i}", [128, 512], F32) for i in range(2)]
        onesf = mk(es, "onesf", [128, 128], F32)
        blkf = mk(es, "blkf", [128, 128], F32)
        pT = [mkp(es, f"pT{i}", [128, 1024], BF16) for i in range(2)]
        pr = [mkp(es, f"pr{i}", [128, 512], F32) for i in range(3)]
        pS1 = mkp(es, "pS1", [128, 512], F32)
        pS2 = mkp(es, "pS2", [128, 512], F32)
        ph = mkp(es, "ph", [128, 512], F32)
        prr = [0]

        def next_pr():
            b = pr[prr[0] % 3]
            prr[0] += 1
            return b

        P.op("dve", lambda e: e.memset(onesf[:, :], 1.0), writes=[onesf])
        P.op("pool", lambda e: e.memset(blkf[:, :], 0.0), writes=[blkf])
        P.op("pool", lambda e: e.memset(blkf[0:64, 0:64], 1.0), reads=[blkf], writes=[blkf])
        P.op("pool", lambda e: e.memset(blkf[64:128, 64:128], 1.0), reads=[blkf], writes=[blkf])
        bcast_load("sp", gmixB, rvec_d[0:1, R_GMIX:R_GMIX + D])

        def head_norm(src, gcol, dst_ap, dst_buf, i2):
            P.op("act", lambda e: e.activation(out=sq[i2][:, :], in_=src[:, :], func=AF.Square), reads=[src], writes=[sq[i2]])
            P.op("pe", lambda e: e.matmul(ph[:, :], lhsT=blkf[:, :], rhs=sq[i2][:, :], start=True, stop=True),
                 reads=[blkf, sq[i2]], writes=[ph])
            P.op("act", lambda e: e.activation(out=rs[i2][:, :], in_=ph[:, :], func=AF.Sqrt, bias=epsT[:, 0:1], scale=1.0 / 64),
                 reads=[ph, epsT], writes=[rs[i2]])
            P.op("dve", lambda e: e.reciprocal(out=rs[i2][:, :], in_=rs[i2][:, :]), reads=[rs[i2]], writes=[rs[i2]])
            P.op("dve", lambda e: e.scalar_tensor_tensor(out=dst_ap, in0=src[:, :], scalar=fvec[:, gcol:gcol + 1], in1=rs[i2][:, :],
                                                         op0=ALU.mult, op1=ALU.mult), reads=[src, fvec, rs[i2]], writes=[dst_buf])

        jcount = 0
        for tt in range(NT5):
            b, ti = divmod(tt, T5B)
            if ti == 0:
                bcast_load("sp", m1B, mod_d[b:b + 1, D:2 * D], reads=[B_mod])
                P.op("dve", lambda e: e.scalar_tensor_tensor(out=m1B[:, :], in0=m1B[:, :], scalar=1.0, in1=gmixB[:, :],
                                                             op0=ALU.add, op1=ALU.mult), reads=[m1B, gmixB], writes=[m1B])
                bcast_load("sp", shmB, mod_d[b:b + 1, 0:D], reads=[B_mod])
                P.op("dve", lambda e: e.memset(uhist[:, :, :], 0.0), reads=[uhist], writes=[uhist])
                P.op("dve", lambda e: e.memset(ghist[:, :, :], 0.0), reads=[ghist], writes=[ghist])
            for s in range(4):
                i2 = s % 2
                xt = xs[i2]
                r0 = tt * 512 + s * 128
                P.dma("sp", lambda e, xt=xt, r0=r0: e.dma_start(out=xt[:, :], in_=x_d[r0:r0 + 128, :]), writes=[xt])
                rms_front((junk, ssq[i2], rstd[i2], hn), xt, m1B, shmB, htm[i2])
                transposes(htm[i2], pT, hT, s, ("act", "dve"))
            for j in range(8):
                wj = win[jcount % 2]
                jcount += 1
                P.dma("pool", lambda e, wj=wj, j=j: e.dma_start(out=wj[:, :, :], in_=win_d[j]), writes=[wj])
                nzf = -(-(NSLOT // 128) // (NT5 * 8))
                for _ in range(nzf):
                    if zf_list:
                        a = zf_list.pop()
                        P.dma("pool", lambda e, a=a: e.dma_start(out=xb_d[a * 128:(a + 1) * 128, :], in_=zt[:, :]), reads=[zt], writes=[B_xb])

                def proj(c, wj=wj):
                    pb = next_pr()

                    def mm(e, pb=pb, c=c, wj=wj):
                        for kc in range(KC):
                            ins = e.matmul(pb[:, :], lhsT=wj[:, kc, c * 128:(c + 1) * 128], rhs=hT[:, kc, :],
                                           start=(kc == 0), stop=(kc == KC - 1))
                        return ins
                    P.op("pe", mm, reads=[wj, hT], writes=[pb])
                    return pb
                i2 = j % 2
                p_xa = proj(0)
                P.op("act", lambda e, p=p_xa, i2=i2: e.copy(out=xa_s[i2][:, :], in_=p[:, :]), reads=[p_xa], writes=[xa_s[i2]])
                p_ba = proj(1)
                p_ca = proj(2)
                u = uw[i2]
                P.op("dve", lambda e, u=u, j=j: e.tensor_copy(out=u[:, 0:2], in_=uhist[:, j, :]), reads=[uhist], writes=[u])
                P.op("dve", lambda e, u=u, p=p_ca, i2=i2: e.tensor_tensor(out=u[:, 2:514], in0=p[:, :], in1=xa_s[i2][:, :], op=ALU.mult),
                     reads=[p_ca, xa_s[i2]], writes=[u])
                P.op("dve", lambda e, u=u, j=j: e.tensor_copy(out=uhist[:, j, :], in_=u[:, 512:514]), reads=[u], writes=[uhist])
                aa = acc_a[i2]
                P.op("dve", lambda e, u=u, aa=aa, j=j: e.tensor_scalar(out=aa[:, :], in0=u[:, 0:512], scalar1=fvec[:, FA_W + j * 3:FA_W + j * 3 + 1],
                                                                       scalar2=None, op0=ALU.mult), reads=[u, fvec], writes=[aa])
                for k in (1, 2):
                    P.op("dve", lambda e, u=u, aa=aa, j=j, k=k: e.scalar_tensor_tensor(
                        out=aa[:, :], in0=u[:, k:k + 512], scalar=fvec[:, FA_W + j * 3 + k:FA_W + j * 3 + k + 1], in1=aa[:, :],
                        op0=ALU.mult, op1=ALU.add), reads=[u, fvec, aa], writes=[aa])
                P.op("dve", lambda e, p=p_ba, aa=aa, i2=i2: e.tensor_tensor(out=ya[i2][:, :], in0=p[:, :], in1=aa[:, :], op=ALU.mult),
                     reads=[p_ba, aa], writes=[ya[i2]])
                head_norm(ya[i2], FHA + j, yT[:, j, :], yT, i2)
                p_vb = proj(3)
                p_gb = proj(4)
                P.op("act", lambda e, p=p_gb, i2=i2: e.activation(out=sg[i2][:, :], in_=p[:, :], func=AF.Sigmoid), reads=[p_gb], writes=[sg[i2]])
                g = gw[i2]
                ceng = "dve"
                P.op(ceng, lambda e, g=g, j=j: e.tensor_copy(out=g[:, 0:30], in_=ghist[:, j, :]), reads=[ghist], writes=[g])
                P.op("dve", lambda e, g=g, p=p_vb, i2=i2: e.tensor_tensor(out=g[:, 30:542], in0=p[:, :], in1=sg[i2][:, :], op=ALU.mult),
                     reads=[p_vb, sg[i2]], writes=[g])
                P.op(ceng, lambda e, g=g, j=j: e.tensor_copy(out=ghist[:, j, :], in_=g[:, 512:542]), reads=[g], writes=[ghist])
                z = zb[j]
                P.op(ceng, lambda e, g=g, z=z, j=j: e.tensor_scalar(out=z[:, :], in0=g[:, 0:512], scalar1=fvec[:, FB_W + j * 31:FB_W + j * 31 + 1],
                                                                    scalar2=fvec[:, FB_B + j:FB_B + j + 1], op0=ALU.mult, op1=ALU.add),
                     reads=[g, fvec], writes=[z])
                for k in range(1, 31):
                    P.op(ceng, lambda e, g=g, z=z, j=j, k=k: e.scalar_tensor_tensor(
                        out=z[:, :], in0=g[:, k:k + 512], scalar=fvec[:, FB_W + j * 31 + k:FB_W + j * 31 + k + 1], in1=z[:, :],
                        op0=ALU.mult, op1=ALU.add), reads=[g, fvec, z], writes=[z])
                P.op("act", lambda e, z=z, i2=i2: e.activation(out=sq[i2][:, :], in_=z[:, :], func=AF.Square), reads=[z], writes=[sq[i2]])
                P.op("pe", lambda e, z=z, j=j: e.matmul(pS1[:, :], lhsT=onesf[:, :], rhs=z[:, :], start=(j == 0), stop=(j == 7)),
                     reads=[onesf, z], writes=[pS1])
                P.op("pe", lambda e, i2=i2, j=j: e.matmul(pS2[:, :], lhsT=onesf[:, :], rhs=sq[i2][:, :], start=(j == 0), stop=(j == 7)),
                     reads=[onesf, sq[i2]], writes=[pS2])
            P.op("act", lambda e: e.activation(out=mean[:, :], in_=pS1[:, :], func=AF.Identity, scale=1.0 / 1024), reads=[pS1], writes=[mean])
            P.op("dve", lambda e: e.tensor_tensor(out=msq[:, :], in0=mean[:, :], in1=mean[:, :], op=ALU.mult), reads=[mean], writes=[msq])
            P.op("dve", lambda e: e.scalar_tensor_tensor(out=msq[:, :], in0=pS2[:, :], scalar=1.0 / 1024, in1=msq[:, :],
                                                         op0=ALU.mult, op1=ALU.subtract), reads=[pS2, msq], writes=[msq])
            P.op("act", lambda e: e.activation(out=lrstd[:, :], in_=msq[:, :], func=AF.Sqrt, bias=epsT[:, 0:1], scale=1.0),
                 reads=[msq, epsT], writes=[lrstd])
            P.op("dve", lambda e: e.reciprocal(out=lrstd[:, :], in_=lrstd[:, :]), reads=[lrstd], writes=[lrstd])
            for j in range(8):
                i2 = j % 2
                z = zb[j]
                P.op("dve", lambda e, z=z, i2=i2: e.tensor_tensor(out=t1[i2][:, :], in0=z[:, :], in1=mean[:, :], op=ALU.subtract),
                     reads=[z, mean], writes=[t1[i2]])
                P.op("pool", lambda e, i2=i2: e.tensor_tensor(out=t1[i2][:, :], in0=t1[i2][:, :], in1=lrstd[:, :], op=ALU.mult),
                     reads=[t1[i2], lrstd], writes=[t1[i2]])
                P.op("act", lambda e, i2=i2, j=j: e.activation(out=zz[i2][:, :], in_=t1[i2][:, :], func=AF.Silu,
                                                               scale=fvec[:, FLN_G + j:FLN_G + j + 1], bias=fvec[:, FLN_B + j:FLN_B + j + 1]),
                     reads=[t1[i2], fvec], writes=[zz[i2]])
                head_norm(zz[i2], FHB + j, yT[:, 8 + j, :], yT, i2)
            P.dma("sp", lambda e, tt=tt: e.dma_start(out=yT_d[tt], in_=yT[:, :, :]), reads=[yT], writes=[B_yT])
        P.flush()

    if phases < 2:
        P.barrier()
        P.flush(final=True)
        top.close()
        return nc

    P.new_phase_sems()
    with ExitStack() as es:
        P.barrier()
        wo = mk(es, "wo", [128, KC, D], BF16)
        gmB = mk(es, "gmB", [128, D], F32)
        yTs = [mk(es, f"yTs{i}", [128, KC, 512], BF16) for i in range(2)]
        xr = [mk(es, f"xr{i}", [128, D], F32) for i in range(3)]
        tmpo = [mk(es, f"tmpo{i}", [128, 512], F32) for i in range(2)]
        po = [mkp(es, f"po{i}", [128, 512], F32) for i in range(4)]
        for q in range(4):
            P.dma("pool", lambda e, q=q: e.dma_start(out=wo[:, q * 4:(q + 1) * 4, :], in_=wout_d[:, q * 4:(q + 1) * 4, :]), writes=[wo])
        cnt = 0
        for tt in range(NT5):
            b, ti = divmod(tt, T5B)
            if ti == 0:
                bcast_load("sp", gmB, mod_d[b:b + 1, 2 * D:3 * D], reads=[B_mod])
            yt = yTs[tt % 2]
            P.dma("sp", lambda e, yt=yt, tt=tt: e.dma_start(out=yt[:, :, :], in_=yT_d[tt]), reads=[B_yT], writes=[yt])
            for s in range(4):
                xt = xr[(tt * 4 + s) % 3]
                r0 = tt * 512 + s * 128
                P.dma("sp", lambda e, xt=xt, r0=r0: e.dma_start(out=xt[:, :], in_=x_d[r0:r0 + 128, :]), writes=[xt])
                for n in range(4):
                    pb = po[cnt % 4]
                    tm = tmpo[cnt % 2]
                    cnt += 1

                    def mm(e, pb=pb, yt=yt, s=s, n=n):
                        for kc in range(KC):
                            ins = e.matmul(pb[:, :], lhsT=yt[:, kc, s * 128:(s + 1) * 128], rhs=wo[:, kc, n * 512:(n + 1) * 512],
                                           start=(kc == 0), stop=(kc == KC - 1))
                        return ins
                    P.op("pe", mm, reads=[yt, wo], writes=[pb])
                    P.op("dve", lambda e, pb=pb, tm=tm, n=n: e.tensor_tensor(out=tm[:, :], in0=pb[:, :], in1=gmB[:, n * 512:(n + 1) * 512], op=ALU.mult),
                         reads=[pb, gmB], writes=[tm])
                    P.op("pool", lambda e, tm=tm, xt=xt, n=n: e.tensor_tensor(out=xt[:, n * 512:(n + 1) * 512], in0=tm[:, :],
                                                                               in1=xt[:, n * 512:(n + 1) * 512], op=ALU.add),
                         reads=[tm, xt], writes=[xt])
                P.dma("sp", lambda e, xt=xt, r0=r0: e.dma_start(out=x1_d[r0:r0 + 128, :], in_=xt[:, :]), reads=[xt], writes=[B_x1])
        P.flush()

    if phases < 3:
        P.barrier()
        P.flush(final=True)
        top.close()
        return nc

    P.new_phase_sems()
    with ExitStack() as es:
        P.barrier()
        m2B = mk(es, "m2B", [128, D], F32)
        shfB = mk(es, "shfB", [128, D], F32)
        gfB = mk(es, "gfB", [128, D], F32)
        x1s = [mk(es, f"x1s{i}", [128, D], F32) for i in range(4)]
        hn = mk(es, "hn2", [128, D], F32)
        junk = mk(es, "junk2", [128, D], BF16)
        h2tm = [mk(es, f"h2tm{i}", [128, D], BF16) for i in range(5)]
        ssq = [mk(es, f"ssq2{i}", [128, 1], F32) for i in range(2)]
        rstd = [mk(es, f"rstd2{i}", [128, 1], F32) for i in range(2)]
        h2T = mk(es, "h2T", [128, KC, 512], BF16)
        wsg = mk(es, "wsg", [128, KC, FE], BF16)
        wsu = mk(es, "wsu", [128, KC, FE], BF16)
        wsd = mk(es, "wsd", [128, 4, D], BF16)
        wrb = mk(es, "wrb", [128, KC, NE], BF16)
        AT = mk(es, "AT", [128, 4, 512], BF16)
        sgs = [mk(es, f"sgs{i}", [128, 512], F32) for i in range(2)]
        tmpo = [mk(es, f"tmp2o{i}", [128, 512], F32) for i in range(2)]
        selall = mk(es, "selall", [128, NT, NE], BF16)
        onesb = mk(es, "onesb", [128, 128], BF16)
        trib = mk(es, "trib", [128, 128], BF16)
        trif = mk(es, "trif", [128, 128], F32)
        rbB = mk(es, "rbB", [128, NE], F32)
        eC1 = mk(es, "eC1", [128, NE], F32)
        eCi = mk(es, "eCi", [128, NE], I32)
        R = {n: mk(es, "r_" + n, [128, NE], F32) for n in ("sc", "bi", "eq", "t", "mk", "masked", "sel", "ws", "lt", "g2", "selv", "pf", "jk")}
        R8 = {n: mk(es, "r8_" + n, [128, 8], F32) for n in ("m1", "m2", "gs", "g8", "gm", "off", "m8", "p8")}
        ssum = mk(es, "ssum", [128, 1], F32)
        pT = [mkp(es, f"pT2{i}", [128, 1024], BF16) for i in range(2)]
        pgu = [mkp(es, f"pgu{i}", [128, 512], F32) for i in range(4)]
        plog = mkp(es, "plog", [128, 512], F32)
        prk = mkp(es, "prk", [128, 512], F32)

        P.dma("pool", lambda e: e.dma_start(out=wsg[:, :, :], in_=wsg_d[:, :, :]), writes=[wsg])
        P.dma("pool", lambda e: e.dma_start(out=wsu[:, :, :], in_=wsu_d[:, :, :]), writes=[wsu])
        P.dma("pool", lambda e: e.dma_start(out=wsd[:, :, :], in_=wsd_d[:, :, :]), writes=[wsd])
        P.dma("pool", lambda e: e.dma_start(out=wrb[:, :, :], in_=wr_d[:, :, :]), writes=[wrb])
        bcast_load("sp", rbB, rvec_d[0:1, R_RB:R_RB + NE])
        P.op("dve", lambda e: e.memset(onesb[:, :], 1.0), writes=[onesb])
        trii = mk(es, "trii", [128, 128], I32)
        P.op("pool", lambda e: e.iota(trii[:, :], pattern=[[1, 128]], base=0, channel_multiplier=-1), writes=[trii])
        P.op("dve", lambda e: e.tensor_copy(out=trif[:, :], in_=trii[:, :]), reads=[trii], writes=[trif])
        P.op("dve", lambda e: e.tensor_scalar(out=trib[:, :], in0=trif[:, :], scalar1=0.0, scalar2=None, op0=ALU.is_gt), reads=[trif], writes=[trib])
        P.op("pool", lambda e: e.iota(eCi[:, :], pattern=[[C, NE]], base=1, channel_multiplier=0), writes=[eCi])
        P.op("dve", lambda e: e.tensor_copy(out=eC1[:, :], in_=eCi[:, :]), reads=[eCi], writes=[eC1])

        def v3(buf):
            return buf[:, :].rearrange("p (g e) -> p g e", g=8)

        def b3(buf8):
            return buf8[:, :].unsqueeze(2).to_broadcast([128, 8, 8])

        cnt = 0
        for tt in range(NT5):
            b, ti = divmod(tt, T5B)
            if ti == 0:
                bcast_load("sp", m2B, mod_d[b:b + 1, 4 * D:5 * D], reads=[B_mod])
                bcast_load("sp", hn, rvec_d[0:1, R_GFFN:R_GFFN + D])
                P.op("dve", lambda e: e.scalar_tensor_tensor(out=m2B[:, :], in0=m2B[:, :], scalar=1.0, in1=hn[:, :],
                                                             op0=ALU.add, op1=ALU.mult), reads=[m2B, hn], writes=[m2B])
                bcast_load("sp", shfB, mod_d[b:b + 1, 3 * D:4 * D], reads=[B_mod])
                bcast_load("sp", gfB, mod_d[b:b + 1, 5 * D:6 * D], reads=[B_mod])
            for s in range(4):
                tile = tt * 4 + s
                i2 = s % 2
                xt = x1s[s]
                r0 = tile * 128
                P.dma("sp", lambda e, xt=xt, r0=r0: e.dma_start(out=xt[:, :], in_=x1_d[r0:r0 + 128, :]), reads=[B_x1], writes=[xt])
                h2 = h2tm[tile % 5]
                rms_front((junk, ssq[i2], rstd[i2], hn), xt, m2B, shfB, h2)
                transposes(h2, pT, h2T, s, ("act", "dve"))

                def mmr(e, s=s):
                    for kc in range(KC):
                        ins = e.matmul(plog[:, 0:NE], lhsT=h2T[:, kc, s * 128:(s + 1) * 128], rhs=wrb[:, kc, :],
                                       start=(kc == 0), stop=(kc == KC - 1))
                    return ins
                P.op("pe", mmr, reads=[h2T, wrb], writes=[plog])
                sc, bi, eq, t_, mkb, masked, sel, ws, lt, g2, selv, pf, jk = (R[n] for n in ("sc", "bi", "eq", "t", "mk", "masked", "sel", "ws", "lt", "g2", "selv", "pf", "jk"))
                m1, m2, gs, g8, gm, off, m8, p8 = (R8[n] for n in ("m1", "m2", "gs", "g8", "gm", "off", "m8", "p8"))
                P.op("act", lambda e: e.activation(out=sc[:, :], in_=plog[:, 0:NE], func=AF.Sigmoid), reads=[plog], writes=[sc])
                P.op("dve", lambda e: e.tensor_tensor(out=bi[:, :], in0=sc[:, :], in1=rbB[:, :], op=ALU.add), reads=[sc, rbB], writes=[bi])
                P.op("dve", lambda e: e.tensor_reduce(out=m1[:, :], in_=v3(bi), axis=AX.X, op=ALU.max), reads=[bi], writes=[m1])
                P.op("dve", lambda e: e.tensor_tensor(out=v3(eq), in0=v3(bi), in1=b3(m1), op=ALU.is_equal), reads=[bi, m1], writes=[eq])
                P.op("dve", lambda e: e.scalar_tensor_tensor(out=t_[:, :], in0=eq[:, :], scalar=-4.0, in1=bi[:, :], op0=ALU.mult, op1=ALU.add),
                     reads=[eq, bi], writes=[t_])
                P.op("dve", lambda e: e.tensor_reduce(out=m2[:, :], in_=v3(t_), axis=AX.X, op=ALU.max), reads=[t_], writes=[m2])
                P.op("dve", lambda e: e.tensor_tensor(out=gs[:, :], in0=m1[:, :], in1=m2[:, :], op=ALU.add), reads=[m1, m2], writes=[gs])
                P.op("dve", lambda e: e.max(out=g8[:, :], in_=gs[:, :]), reads=[gs], writes=[g8])
                P.op("dve", lambda e: e.tensor_scalar(out=gm[:, :], in0=gs[:, :], scalar1=g8[:, 3:4], scalar2=None, op0=ALU.is_ge),
                     reads=[gs, g8], writes=[gm])
                P.op("dve", lambda e: e.tensor_tensor(out=v3(mkb), in0=v3(bi), in1=b3(gm), op=ALU.mult), reads=[bi, gm], writes=[mkb])
                P.op("dve", lambda e: e.tensor_scalar(out=off[:, :], in0=gm[:, :], scalar1=4.0, scalar2=-4.0, op0=ALU.mult, op1=ALU.add),
                     reads=[gm], writes=[off])
                P.op("dve", lambda e: e.tensor_tensor(out=v3(masked), in0=v3(mkb), in1=b3(off), op=ALU.add), reads=[mkb, off], writes=[masked])
                P.op("dve", lambda e: e.max(out=m8[:, :], in_=masked[:, :]), reads=[masked], writes=[m8])
                P.op("dve", lambda e: e.tensor_scalar(out=sel[:, :], in0=masked[:, :], scalar1=m8[:, 7:8], scalar2=None, op0=ALU.is_ge),
                     reads=[masked, m8], writes=[sel])
                P.op("dve", lambda e, tile=tile: e.tensor_copy(out=selall[:, tile, :], in_=sel[:, :]), reads=[sel], writes=[selall])
                P.op("dve", lambda e: e.memset(ssum[:, :], 0.0), reads=[ssum], writes=[ssum])
                P.op("dve", lambda e: e.scalar_tensor_tensor(out=ws[:, :], in0=sc[:, :], scalar=1.0, in1=sel[:, :], op0=ALU.mult, op1=ALU.mult,
                                                             accum_out=ssum[:, 0:1]), reads=[sc, sel, ssum], writes=[ws, ssum])
                P.op("dve", lambda e: e.reciprocal(out=ssum[:, :], in_=ssum[:, :]), reads=[ssum], writes=[ssum])
                P.op("dve", lambda e: e.tensor_scalar(out=ssum[:, :], in0=ssum[:, :], scalar1=2.5, scalar2=None, op0=ALU.mult),
                     reads=[ssum], writes=[ssum])

                def mrk(e, tile=tile):
                    for jt in range(tile):
                        e.matmul(prk[:, 0:NE], lhsT=onesb[:, :], rhs=selall[:, jt, :], start=(jt == 0), stop=False)
                    return e.matmul(prk[:, 0:NE], lhsT=trib[:, :], rhs=selall[:, tile, :], start=(tile == 0), stop=True)
                P.op("pe", mrk, reads=[onesb, trib, selall], writes=[prk])
                P.op("dve", lambda e: e.tensor_scalar(out=lt[:, :], in0=prk[:, 0:NE], scalar1=float(C), scalar2=None, op0=ALU.is_lt),
                     reads=[prk], writes=[lt])
                P.op("dve", lambda e: e.scalar_tensor_tensor(out=g2[:, :], in0=ws[:, :], scalar=ssum[:, 0:1], in1=lt[:, :], op0=ALU.mult, op1=ALU.mult),
                     reads=[ws, ssum, lt], writes=[g2])
                P.op("dve", lambda e: e.tensor_tensor(out=selv[:, :], in0=sel[:, :], in1=lt[:, :], op=ALU.mult), reads=[sel, lt], writes=[selv])
                P.op("dve", lambda e: e.tensor_tensor(out=pf[:, :], in0=prk[:, 0:NE], in1=eC1[:, :], op=ALU.add), reads=[prk, eC1], writes=[pf])
                P.op("dve", lambda e: e.tensor_tensor(out=pf[:, :], in0=pf[:, :], in1=selv[:, :], op=ALU.mult), reads=[pf, selv], writes=[pf])
                P.op("dve", lambda e: e.tensor_scalar(out=pf[:, :], in0=pf[:, :], scalar1=-1.0, scalar2=None, op0=ALU.add), reads=[pf], writes=[pf])
                P.op("dve", lambda e: e.max(out=p8[:, :], in_=pf[:, :]), reads=[pf], writes=[p8])
                P.op("dve", lambda e: e.tensor_scalar(out=R8["m2"][:, :], in0=p8[:, :], scalar1=0.0, scalar2=None, op0=ALU.is_lt), reads=[p8], writes=[R8["m2"]])
                P.op("dve", lambda e: e.scalar_tensor_tensor(out=R8["m1"][:, :], in0=R8["m2"][:, :], scalar=600000.0, in1=p8[:, :], op0=ALU.mult, op1=ALU.add),
                     reads=[R8["m2"], p8], writes=[R8["m1"]])
                P.op("dve", lambda e, tile=tile: e.tensor_copy(out=posall[:, tile * 8:(tile + 1) * 8], in_=R8["m1"][:, :]), reads=[R8["m1"]], writes=[posall])
                for k in range(8):
                    P.op("dve", lambda e, tile=tile, k=k: e.scalar_tensor_tensor(
                        out=jk[:, :], in0=pf[:, :], scalar=p8[:, k:k + 1], in1=g2[:, :], op0=ALU.is_equal, op1=ALU.mult,
                        accum_out=wall[:, tile * 8 + k:tile * 8 + k + 1]), reads=[pf, p8, g2, wall], writes=[jk, wall])
                for k in range(8):
                    P.dma("pool", lambda e, h2=h2, tile=tile, k=k: e.indirect_dma_start(
                        out=xb_d[:, :], out_offset=bass.IndirectOffsetOnAxis(ap=posall[:, tile * 8 + k:tile * 8 + k + 1], axis=0),
                        in_=h2[:, :], in_offset=None, bounds_check=get_bc(e, 2), oob_is_err=False),
                        reads=[h2, posall, B_xb], writes=[B_xb])
                if dbg:
                    P.dma("sp", lambda e, r0=r0, tile=tile: e.dma_start(out=dbg_d[r0:r0 + 128, 8:16], in_=wall[:, tile * 8:(tile + 1) * 8]),
                          reads=[wall], writes=[B_out])
                    P.dma("sp", lambda e, r0=r0: e.dma_start(out=dbg_d[r0:r0 + 128, 0:8], in_=R8["p8"][:, :]),
                          reads=[R8["p8"]], writes=[B_out])
            for fc in range(4):
                pg = pgu[(2 * fc) % 4]
                pu = pgu[(2 * fc + 1) % 4]

                def mmg(e, pg=pg, fc=fc, w=wsg):
                    for kc in range(KC):
                        ins = e.matmul(pg[:, :], lhsT=w[:, kc, fc * 128:(fc + 1) * 128], rhs=h2T[:, kc, :], start=(kc == 0), stop=(kc == KC - 1))
                    return ins

                def mmu(e, pu=pu, fc=fc, w=wsu):
                    for kc in range(KC):
                        ins = e.matmul(pu[:, :], lhsT=w[:, kc, fc * 128:(fc + 1) * 128], rhs=h2T[:, kc, :], start=(kc == 0), stop=(kc == KC - 1))
                    return ins
                P.op("pe", mmg, reads=[wsg, h2T], writes=[pg])
                P.op("pe", mmu, reads=[wsu, h2T], writes=[pu])
                sgb = sgs[fc % 2]
                P.op("act", lambda e, pg=pg, sgb=sgb: e.activation(out=sgb[:, :], in_=pg[:, :], func=AF.Silu), reads=[pg], writes=[sgb])
                P.op("dve", lambda e, pu=pu, sgb=sgb, fc=fc: e.tensor_tensor(out=AT[:, fc, :], in0=pu[:, :], in1=sgb[:, :], op=ALU.mult),
                     reads=[pu, sgb], writes=[AT])
            for s in range(4):
                xt = x1s[s]
                r0 = (tt * 4 + s) * 128
                for n in range(4):
                    pb = pgu[cnt % 4]
                    tm = tmpo[cnt % 2]
                    cnt += 1

                    def mmd(e, pb=pb, s=s, n=n):
                        for fc in range(4):
                            ins = e.matmul(pb[:, :], lhsT=AT[:, fc, s * 128:(s + 1) * 128], rhs=wsd[:, fc, n * 512:(n + 1) * 512],
                                           start=(fc == 0), stop=(fc == 3))
                        return ins
                    P.op("pe", mmd, reads=[AT, wsd], writes=[pb])
                    P.op("dve", lambda e, pb=pb, tm=tm, n=n: e.tensor_tensor(out=tm[:, :], in0=pb[:, :], in1=gfB[:, n * 512:(n + 1) * 512], op=ALU.mult),
                         reads=[pb, gfB], writes=[tm])
                    P.op("pool", lambda e, tm=tm, xt=xt, n=n: e.tensor_tensor(out=xt[:, n * 512:(n + 1) * 512], in0=tm[:, :],
                                                                               in1=xt[:, n * 512:(n + 1) * 512], op=ALU.add),
                         reads=[tm, xt], writes=[xt])
                P.dma("sp", lambda e, xt=xt, r0=r0: e.dma_start(out=x2_d[r0:r0 + 128, :], in_=xt[:, :]), reads=[xt], writes=[B_x2])

        def mcnt(e):
            for jt in range(NT):
                ins = e.matmul(prk[0:1, 0:NE], lhsT=onesb[:, 0:1], rhs=selall[:, jt, :], start=(jt == 0), stop=(jt == NT - 1))
            return ins
        P.op("pe", mcnt, reads=[onesb, selall], writes=[prk])
        P.cnt_ev = P.op("dve", lambda e: e.tensor_copy(out=cnts[0:1, :], in_=prk[0:1, 0:NE]), reads=[prk], writes=[cnts])
        P.cnt_ap = lambda idx: cnts[0:1, idx:idx + 1]
        P.flush()

    if phases < 4:
        P.barrier()
        P.flush(final=True)
        top.close()
        return nc

    P.new_phase_sems()
    with ExitStack() as es:
        P.barrier()
        wg = [mk(es, f"wg{i}", [128, KC, FE], BF16) for i in range(2)]
        wu = [mk(es, f"wu{i}", [128, KC, FE], BF16) for i in range(2)]
        wd = [mk(es, f"wd{i}", [128, 4, D], BF16) for i in range(2)]
        xrow = [mk(es, f"xrow{i}", [128, D], BF16) for i in range(4)]
        XT = [mk(es, f"XT{i}", [128, KC, 512], BF16) for i in range(2)]
        ATe = [mk(es, f"ATe{i}", [128, 4, 512], BF16) for i in range(2)]
        sge = [mk(es, f"sge{i}", [128, 512], F32) for i in range(2)]
        yst = [mk(es, f"yst{i}", [128, D], BF16) for i in range(3)]
        pT = [mkp(es, f"pT3{i}", [128, 1024], BF16) for i in range(2)]
        pgu = [mkp(es, f"pgu3{i}", [128, 512], F32) for i in range(4)]
        py = [mkp(es, f"py3{i}", [128, 512], F32) for i in range(2)]
        gcnt = 0
        rowc = 0
        ycnt = 0
        yscnt = 0

        def load_w(e_):
            i = e_ % 2
            P.dma("pool", lambda e: e.dma_start(out=wg[i][:, :, :], in_=wg_d[e_]), writes=[wg[i]])
            P.dma("pool", lambda e: e.dma_start(out=wu[i][:, :, :], in_=wu_d[e_]), writes=[wu[i]])
            P.dma("pool", lambda e: e.dma_start(out=wd[i][:, :, :], in_=wd_d[e_]), writes=[wd[i]])

        load_w(0)
        for ex in range(NE):
            if ex + 1 < NE:
                load_w(ex + 1)
            wi = ex % 2
            for (g0, gn) in groups:
                P.begin_cond(ex, g0)
                xt_ = XT[gcnt % 2]
                at_ = ATe[gcnt % 2]
                gcnt += 1
                ns = gn // 128
                for s in range(ns):
                    xr_ = xrow[rowc % 4]
                    rowc += 1
                    r0 = ex * C + g0 + s * 128
                    P.dma("sp", lambda e, xr_=xr_, r0=r0: e.dma_start(out=xr_[:, :], in_=xb_d[r0:r0 + 128, :]), reads=[B_xb], writes=[xr_])
                    transposes(xr_, pT, xt_, s, ("act", "dve"))
                for fc in range(4):
                    pg = pgu[(2 * fc) % 4]
                    pu = pgu[(2 * fc + 1) % 4]

                    def mmg(e, pg=pg, fc=fc, w=wg[wi], xt_=xt_, gn=gn):
                        for kc in range(KC):
                            ins = e.matmul(pg[:, 0:gn], lhsT=w[:, kc, fc * 128:(fc + 1) * 128], rhs=xt_[:, kc, 0:gn], start=(kc == 0), stop=(kc == KC - 1))
                        return ins

                    def mmu(e, pu=pu, fc=fc, w=wu[wi], xt_=xt_, gn=gn):
                        for kc in range(KC):
                            ins = e.matmul(pu[:, 0:gn], lhsT=w[:, kc, fc * 128:(fc + 1) * 128], rhs=xt_[:, kc, 0:gn], start=(kc == 0), stop=(kc == KC - 1))
                        return ins
                    P.op("pe", mmg, reads=[wg[wi], xt_], writes=[pg])
                    P.op("pe", mmu, reads=[wu[wi], xt_], writes=[pu])
                    sgb = sge[fc % 2]
                    P.op("act", lambda e, pg=pg, sgb=sgb, gn=gn: e.activation(out=sgb[:, 0:gn], in_=pg[:, 0:gn], func=AF.Silu), reads=[pg], writes=[sgb])
                    P.op("dve", lambda e, pu=pu, sgb=sgb, fc=fc, at_=at_, gn=gn: e.tensor_tensor(out=at_[:, fc, 0:gn], in0=pu[:, 0:gn], in1=sgb[:, 0:gn], op=ALU.mult),
                         reads=[pu, sgb], writes=[at_])
                for s in range(ns):
                    ys = yst[yscnt % 3]
                    yscnt += 1
                    r0 = ex * C + g0 + s * 128
                    for n in range(4):
                        pb = py[ycnt % 2]
                        ycnt += 1

                        def mmd(e, pb=pb, s=s, n=n, at_=at_, w=wd[wi]):
                            for fc in range(4):
                                ins = e.matmul(pb[:, :], lhsT=at_[:, fc, s * 128:(s + 1) * 128], rhs=w[:, fc, n * 512:(n + 1) * 512],
                                               start=(fc == 0), stop=(fc == 3))
                            return ins
                        P.op("pe", mmd, reads=[at_, wd[wi]], writes=[pb])
                        if n % 2 == 0:
                            P.op("act", lambda e, pb=pb, ys=ys, n=n: e.copy(out=ys[:, n * 512:(n + 1) * 512], in_=pb[:, :]), reads=[pb], writes=[ys])
                        else:
                            P.op("dve", lambda e, pb=pb, ys=ys, n=n: e.tensor_copy(out=ys[:, n * 512:(n + 1) * 512], in_=pb[:, :]), reads=[pb], writes=[ys])
                    P.dma("sp", lambda e, ys=ys, r0=r0: e.dma_start(out=yb_d[r0:r0 + 128, :], in_=ys[:, :]), reads=[ys], writes=[B_yb])
                P.end_cond()
        P.flush()

    if phases < 5:
        P.barrier()
        P.flush(final=True)
        top.close()
        return nc

    P.new_phase_sems()
    with ExitStack() as es:
        P.barrier()
        NG = 16
        gsl = [mk(es, f"gsl{i}", [128, D], BF16) for i in range(NG)]
        gfB = mk(es, "gfB4", [128, D], F32)
        gfinB = mk(es, "gfinB", [128, D], F32)
        x2s = [mk(es, f"x2s{i}", [128, D], F32) for i in range(2)]
        acc = [mk(es, f"acc4{i}", [128, D], F32) for i in range(2)]
        junk = mk(es, "junk4", [128, D], BF16)
        ssq = [mk(es, f"ssq4{i}", [128, 1], F32) for i in range(2)]
        rstd = [mk(es, f"rstd4{i}", [128, 1], F32) for i in range(2)]
        for i in range(NG):
            P.op("pool" if i % 2 else "dve", lambda e, i=i: e.memset(gsl[i][:, :], 0.0), writes=[gsl[i]])
        bcast_load("sp", gfinB, rvec_d[0:1, R_GFIN:R_GFIN + D])
        gc = 0
        gather_evs = []
        for tile in range(NT):
            b = (tile * 128) // S
            if (tile * 128) % S == 0:
                bcast_load("sp", gfB, mod_d[b:b + 1, 5 * D:6 * D], reads=[B_mod])
            i2 = tile % 2
            r0 = tile * 128
            xt = x2s[i2]
            P.dma("sp", lambda e, xt=xt, r0=r0: e.dma_start(out=xt[:, :], in_=x2_d[r0:r0 + 128, :]), reads=[B_x2], writes=[xt])
            a = acc[i2]
            for k in range(8):
                gt = gsl[gc % NG]
                gc += 1
                if True:
                  P.dma("pool", lambda e, gt=gt, tile=tile, k=k: e.indirect_dma_start(
                    out=gt[:, :], out_offset=None, in_=yb_d[:, :],
                    in_offset=bass.IndirectOffsetOnAxis(ap=posall[:, tile * 8 + k:tile * 8 + k + 1], axis=0),
                    bounds_check=get_bc(e, 4), oob_is_err=False), reads=[B_yb, posall, gt], writes=[gt],
                    extra=[gather_evs[-GSER]] if len(gather_evs) >= GSER else [])
                  gather_evs.append(gt.lw)
                wcol = wall[:, tile * 8 + k:tile * 8 + k + 1]
                if k == 0:
                    P.op("dve", lambda e, gt=gt, a=a, wcol=wcol: e.tensor_scalar(out=a[:, :], in0=gt[:, :], scalar1=wcol, scalar2=None, op0=ALU.mult),
                         reads=[gt, wall], writes=[a])
                else:
                    P.op("dve", lambda e, gt=gt, a=a, wcol=wcol: e.scalar_tensor_tensor(out=a[:, :], in0=gt[:, :], scalar=wcol, in1=a[:, :],
                                                                                         op0=ALU.mult, op1=ALU.add), reads=[gt, wall, a], writes=[a])
            P.op("pool", lambda e, a=a: e.tensor_tensor(out=a[:, :], in0=a[:, :], in1=gfB[:, :], op=ALU.mult), reads=[a, gfB], writes=[a])
            P.op("pool", lambda e, a=a, xt=xt: e.tensor_tensor(out=xt[:, :], in0=a[:, :], in1=xt[:, :], op=ALU.add), reads=[a, xt], writes=[xt])
            P.op("act", lambda e, xt=xt, i2=i2: e.activation(out=junk[:, :], in_=xt[:, :], func=AF.Square, accum_out=ssq[i2][:, 0:1]),
                 reads=[xt], writes=[junk, ssq[i2]])
            P.op("act", lambda e, i2=i2: e.activation(out=rstd[i2][:, 0:1], in_=ssq[i2][:, 0:1], func=AF.Sqrt, bias=epsT[:, 0:1], scale=1.0 / D),
                 reads=[ssq[i2], epsT], writes=[rstd[i2]])
            P.op("dve", lambda e, i2=i2: e.reciprocal(out=rstd[i2][:, 0:1], in_=rstd[i2][:, 0:1]), reads=[rstd[i2]], writes=[rstd[i2]])
            P.op("dve", lambda e, xt=xt, a=a, i2=i2: e.scalar_tensor_tensor(out=a[:, :], in0=xt[:, :], scalar=rstd[i2][:, 0:1], in1=gfinB[:, :],
                                                                            op0=ALU.mult, op1=ALU.mult), reads=[xt, rstd[i2], gfinB], writes=[a])
            P.dma("sp", lambda e, a=a, r0=r0: e.dma_start(out=out_d[r0:r0 + 128, :], in_=a[:, :]), reads=[a], writes=[B_out])
        P.flush(final=True)
    top.close()
    return nc


def prep_shared(inp):
    f32 = np.float32
    sh = {}
    sh["w_ada"] = np.ascontiguousarray(inp["w_ada"][0].reshape(KC, 128, 6 * D).transpose(1, 0, 2))
    W = inp["w_in"][0]
    sh["w_in"] = np.ascontiguousarray(W.reshape(KC, 128, 5, 8, 128).transpose(3, 1, 0, 2, 4).reshape(8, 128, KC, 640))
    sh["w_out"] = np.ascontiguousarray(inp["w_out"][0].reshape(KC, 128, D).transpose(1, 0, 2))
    sh["w_router"] = np.ascontiguousarray(inp["w_router"][0].reshape(KC, 128, NE).transpose(1, 0, 2))
    sh["ws_gate"] = np.ascontiguousarray(inp["w_shared_gate"][0].reshape(KC, 128, FE).transpose(1, 0, 2))
    sh["ws_up"] = np.ascontiguousarray(inp["w_shared_up"][0].reshape(KC, 128, FE).transpose(1, 0, 2))
    sh["ws_down"] = np.ascontiguousarray(inp["w_shared_down"][0].reshape(4, 128, D).transpose(1, 0, 2))
    sh["w_gate"] = np.ascontiguousarray(inp["w_gate"][0].reshape(NE, KC, 128, FE).transpose(0, 2, 1, 3))
    sh["w_up"] = np.ascontiguousarray(inp["w_up"][0].reshape(NE, KC, 128, FE).transpose(0, 2, 1, 3))
    sh["w_down"] = np.ascontiguousarray(inp["w_down"][0].reshape(NE, 4, 128, D).transpose(0, 2, 1, 3))
    fv = np.zeros((128, NF), f32)
    fv[:, FA_W:FA_W + 24] = inp["conv_a_w"][0].reshape(3, 8, 128).transpose(2, 1, 0).reshape(128, 24)
    fv[:, FB_W:FB_W + 248] = inp["conv_b_w"][0].reshape(31, 8, 128).transpose(2, 1, 0).reshape(128, 248)
    for off, key in ((FB_B, "conv_b_b"), (FLN_G, "ln_b_g"), (FLN_B, "ln_b_b"), (FHA, "head_norm_a_g"), (FHB, "head_norm_b_g")):
        fv[:, off:off + 8] = inp[key][0].reshape(8, 128).T
    sh["fvec"] = fv
    rv = np.zeros((1, NR), f32)
    rv[0, R_GMIX:R_GMIX + D] = inp["norm_mix_g"][0]
    rv[0, R_GFFN:R_GFFN + D] = inp["norm_ffn_g"][0]
    rv[0, R_GFIN:R_GFIN + D] = inp["norm_final_g"]
    rv[0, R_RB:R_RB + NE] = inp["router_bias"][0]
    rv[0, R_BADA:R_BADA + 6 * D] = inp["b_ada"][0]
    sh["rvec"] = rv
    return sh


def core_inputs(sh, x_core, c_core):
    NB = c_core.shape[0]
    m = dict(sh)
    m["x"] = np.ascontiguousarray(x_core.reshape(-1, D))
    m["cT"] = np.ascontiguousarray(c_core.reshape(NB, KC, 128).transpose(2, 1, 0))
    return m


CAP = 1024
GSER = 1


def kernel(**inputs):
    inp = {k: np.asarray(v) for k, v in inputs.items()}
    x = inp["x"]
    B, S, _ = x.shape
    NB = B // NCORES
    sh = prep_shared(inp)
    in_maps = [core_inputs(sh, x[i * NB:(i + 1) * NB], inp["c"][i * NB:(i + 1) * NB]) for i in range(NCORES)]
    nc = build(NB, S, CAP)
    res = run_bass_kernel_spmd(nc, in_maps, core_ids=list(range(NCORES)))
    out = np.concatenate([r["out"].reshape(NB, S, D) for r in res.results], axis=0)
    return out.astype(np.float32)
```

```python
import os
import numpy as np
from contextlib import ExitStack
import concourse.bass as bass
import concourse.mybir as mybir
from concourse.bass_utils import run_bass_kernel_spmd

F32 = mybir.dt.float32
BF16 = mybir.dt.bfloat16
I32 = mybir.dt.int32
ALU = mybir.AluOpType
AF = mybir.ActivationFunctionType
AX = mybir.AxisListType

D = 2048
KC = 16
NE = 64
FE = 512
EPS = 1e-6
NCORES = 8

ENGS = ("pe", "act", "dve", "pool", "sp")


class Buf:
    __slots__ = ("name", "t", "lw", "rd")

    def __init__(self, name, t=None):
        self.name = name
        self.t = t
        self.lw = None
        self.rd = []

    def __getitem__(self, k):
        return self.t[k]


class Prog:
    def __init__(self, nc, n_dma_sems=12):
        self.nc = nc
        self.es = ExitStack()
        self.eng = {"pe": nc.tensor, "act": nc.scalar, "dve": nc.vector,
                    "pool": nc.gpsimd, "sp": nc.sync}
        self.ops = {e: [] for e in ENGS}
        self.waited = {e: {} for e in ENGS}
        self.psem = {}
        self.pcnt = {}
        self.n_dma_sems = n_dma_sems
        self.dsem = {}
        self.dcnt = {}
        self.dlast = {}
        self.drr = {}
        self.last_ev = {e: None for e in ENGS}
        self.gen = 0
        self.cond = None
        self.cregs = {}
        self.cnt_ap = None
        self.cnt_ev = None
        for q in ("sp", "pool", "act"):
            self.dsem[q] = [self._sem(f"d_{q}{i}") for i in range(n_dma_sems)]
            self.dcnt[q] = [0] * n_dma_sems
            self.dlast[q] = [None] * n_dma_sems
            self.drr[q] = 0
        self.new_phase_sems()

    def cond_reg(self, e, engine, idx):
        st = self.cregs.get(e)
        if st is None:
            st = {"reg": engine.alloc_register(f"cnt_{e}"), "idx": None}
            self.cregs[e] = st
        if st["idx"] != idx:
            engine.wait_ge(self.cnt_ev[0], self.cnt_ev[1])
            engine.reg_load(st["reg"], self.cnt_ap(idx))
            st["idx"] = idx
        return st["reg"]

    def _sem(self, name):
        return self.es.enter_context(self.nc.semaphore(name))

    def new_phase_sems(self):
        self.gen += 1
        for e in ("pe", "act", "dve", "pool"):
            self.psem[e] = self._sem(f"p{self.gen}_{e}")
            self.pcnt[e] = 0

    def _need(self, e, evs):
        w = self.waited[e]
        best = {}
        for ev in evs:
            if ev is None:
                continue
            s, v = ev
            if w.get(id(s), 0) >= v:
                continue
            if id(s) not in best or best[id(s)][1] < v:
                best[id(s)] = (s, v)
        out = []
        for k, (s, v) in best.items():
            w[k] = v
            out.append((s, v))
        return out

    @staticmethod
    def _deps(reads, writes):
        evs = []
        for b in reads:
            evs.append(b.lw)
        for b in writes:
            evs.append(b.lw)
            evs.extend(b.rd)
        return evs

    @staticmethod
    def _commit(ev, reads, writes):
        for b in reads:
            b.rd.append(ev)
        for b in writes:
            b.lw = ev
            b.rd = []

    def begin_cond(self, idx, thr):
        self.cond = (idx, thr)
        self.cond_snap = {e: dict(self.waited[e]) for e in ENGS}

    def end_cond(self):
        self.cond = None
        for e in ENGS:
            self.waited[e] = self.cond_snap[e]

    def op(self, e, fn, reads=(), writes=(), extra=()):
        evs = self._deps(reads, writes) + list(extra)
        waits = self._need(e, evs)
        self.pcnt[e] += 1
        ev = (self.psem[e], self.pcnt[e])
        self.ops[e].append((waits, fn, ev, 1, self.cond, None))
        self._commit(ev, reads, writes)
        self.last_ev[e] = ev
        return ev

    def dma(self, q, fn, reads=(), writes=(), extra=()):
        i = self.drr[q]
        self.drr[q] = (i + 1) % self.n_dma_sems
        evs = self._deps(reads, writes) + list(extra) + [self.dlast[q][i]]
        waits = self._need(q, evs)
        prev = self.dcnt[q][i]
        self.dcnt[q][i] += 16
        ev = (self.dsem[q][i], self.dcnt[q][i])
        self.dlast[q][i] = ev
        self.ops[q].append((waits, fn, ev, 16, self.cond, prev))
        self._commit(ev, reads, writes)
        return ev

    def barrier(self):
        evs = [self.last_ev[e] for e in ("pe", "act", "dve", "pool")]
        for q in ("sp", "pool", "act"):
            evs.extend(self.dlast[q])
        for e in ENGS:
            waits = self._need(e, evs)
            if waits:
                self.ops[e].append((waits, None, None, 0, None, None))

    def flush(self, final=False):
        if final:
            self.barrier()
        ops = self.ops

        def emit_one(engine, o):
            waits, fn, ev, inc, _, _ = o
            for s, v in waits:
                engine.wait_ge(s, v)
            if fn is not None:
                ins = fn(engine)
                ins.then_inc(ev[0], inc)

        def emit(e):
            def body(engine):
                lst = ops[e]
                i = 0
                while i < len(lst):
                    c = lst[i][4]
                    if c is None:
                        emit_one(engine, lst[i])
                        i += 1
                        continue
                    j = i
                    while j < len(lst) and lst[j][4] == c:
                        j += 1
                    run = lst[i:j]
                    reg = self.cond_reg(e, engine, c[0])
                    with engine.If_lt(reg, c[1] + 1):
                        for (waits, fn, ev, inc, _, prev) in run:
                            for sm, v in waits:
                                engine.wait_ge(sm, v)
                            if fn is None:
                                continue
                            if inc == 1:
                                if ev[1] > 1:
                                    engine.wait_ge(ev[0], ev[1] - 1)
                                engine.sem_inc(ev[0], 1)
                            else:
                                if prev:
                                    engine.wait_ge(ev[0], prev)
                                engine.sem_inc(ev[0], 16)
                    with engine.Else():
                        for o in run:
                            emit_one(engine, o)
                    i = j
            return body

        with self.nc.Block() as block:
            block.tensor(emit("pe"))
            block.scalar(emit("act"))
            block.vector(emit("dve"))
            block.gpsimd(emit("pool"))
            block.sync(emit("sp"))
        self.ops = {e: [] for e in ENGS}


FA_W = 0
FB_W = 24
FB_B = 24 + 248
FLN_G = FB_B + 8
FLN_B = FLN_G + 8
FHA = FLN_B + 8
FHB = FHA + 8
NF = FHB + 8
R_GMIX = 0
R_GFFN = D
R_GFIN = 2 * D
R_RB = 3 * D
R_BADA = 3 * D + NE
NR = R_BADA + 6 * D


def build(NB, S, C, phases=5, dbg=False):
    TC = NB * S
    NT = TC // 128
    NT5 = TC // 512
    T5B = S // 512
    NSLOT = NE * C
    groups = [(0, C // 2), (C // 2, C // 4), (3 * C // 4, C // 4)]
    assert C // 2 <= 512 and (C // 4) % 128 == 0

    nc = bass.Bass("TRN2", target_bir_lowering=False)
    dt_in = lambda name, shape: nc.dram_tensor(name, shape, F32, kind="ExternalInput").ap()
    x_d = dt_in("x", [TC, D])
    cT_d = dt_in("cT", [128, KC, NB])
    wada_d = dt_in("w_ada", [128, KC, 6 * D])
    rvec_d = dt_in("rvec", [1, NR])
    fvec_d = dt_in("fvec", [128, NF])
    win_d = dt_in("w_in", [8, 128, KC, 640])
    wout_d = dt_in("w_out", [128, KC, D])
    wr_d = dt_in("w_router", [128, KC, NE])
    wsg_d = dt_in("ws_gate", [128, KC, FE])
    wsu_d = dt_in("ws_up", [128, KC, FE])
    wsd_d = dt_in("ws_down", [128, 4, D])
    wg_d = dt_in("w_gate", [NE, 128, KC, FE])
    wu_d = dt_in("w_up", [NE, 128, KC, FE])
    wd_d = dt_in("w_down", [NE, 128, 4, D])
    out_d = nc.dram_tensor("out", [TC, D], F32, kind="ExternalOutput").ap()
    sk = "ExternalOutput" if dbg else "Internal"
    mod_d = nc.dram_tensor("modd", [NB, 6 * D], F32, kind=sk).ap()
    yT_d = nc.dram_tensor("yTd", [NT5, 128, KC, 512], BF16, kind=sk).ap()
    x1_d = nc.dram_tensor("x1d", [TC, D], F32, kind=sk).ap()
    x2_d = nc.dram_tensor("x2d", [TC, D], F32, kind=sk).ap()
    xb_d = nc.dram_tensor("xbuf", [NSLOT, D], BF16, kind="Internal").ap()
    yb_d = nc.dram_tensor("ybuf", [NSLOT, D], BF16, kind="Internal").ap()
    if dbg:
        dbg_d = nc.dram_tensor("dbg", [TC, 16], F32, kind="ExternalOutput").ap()
    B_mod, B_yT, B_x1, B_x2, B_xb, B_yb, B_out = (Buf(n) for n in ("modd", "yTd", "x1d", "x2d", "xbuf", "ybuf", "outd"))

    P = Prog(nc)
    top = ExitStack()
    bc_reg = {}

    def get_bc(e, key):
        if key not in bc_reg:
            r_ = e.alloc_register(f"bc{key}")
            e.reg_mov(r_, NSLOT - 1)
            bc_reg[key] = r_
        return bc_reg[key]

    def mk(es, name, shape, dt):
        return Buf(name, es.enter_context(nc.sbuf_tensor("s_" + name, shape, dt)))

    def mkp(es, name, shape, dt):
        return Buf(name, es.enter_context(nc.psum_tensor("q_" + name, shape, dt)))

    posall = mk(top, "posall", [128, NT * 8], I32)
    wall = mk(top, "wall", [128, NT * 8], F32)
    ident = mk(top, "ident", [128, 128], BF16)
    epsT = mk(top, "epsT", [128, 1], F32)
    fvec = mk(top, "fvec", [128, NF], F32)
    cnts = mk(top, "cnts", [1, NE], I32)
    zt = mk(top, "zt", [128, 2048], BF16)

    with ExitStack() as es:
        identf = mk(es, "identf", [128, 128], F32)
        cT = mk(es, "cT", [128, KC, NB], F32)
        cact = mk(es, "cact", [128, KC, NB], F32)
        wa = [mk(es, f"wa{i}", [128, KC, 512], F32) for i in range(2)]
        modsb = mk(es, "modsb", [NB, 6 * D], F32)
        badaB = mk(es, "badaB", [NB, 6 * D], F32)
        pm = [mkp(es, f"pm{i}", [128, 512], F32) for i in range(2)]

        P.op("pool", lambda e: e.memset(identf[:, :], 0.0), writes=[identf])
        P.op("pool", lambda e: e.affine_select(out=identf[:, :], in_=identf[:, :], pattern=[[-1, 128]],
                                               compare_op=ALU.not_equal, fill=1.0, base=0, channel_multiplier=1),
             reads=[identf], writes=[identf])
        P.op("dve", lambda e: e.tensor_copy(out=ident[:, :], in_=identf[:, :]), reads=[identf], writes=[ident])
        P.op("dve", lambda e: e.memset(epsT[:, :], EPS), writes=[epsT])
        P.op("dve", lambda e: e.memset(wall[:, :], 0.0), writes=[wall])
        P.op("pool", lambda e: e.memset(zt[:, :], 0.0), writes=[zt])
        P.dma("sp", lambda e: e.dma_start(out=fvec[:, :], in_=fvec_d[:, :]), writes=[fvec])
        P.dma("sp", lambda e: e.dma_start(out=cT[:, :, :], in_=cT_d[:, :, :]), writes=[cT])
        P.dma("sp", lambda e: e.dma_start(out=badaB[:, :], in_=rvec_d[0:1, R_BADA:R_BADA + 6 * D].partition_broadcast(NB)),
              writes=[badaB])
        P.op("act", lambda e: e.activation(out=cact[:, :, :], in_=cT[:, :, :], func=AF.Silu), reads=[cT], writes=[cact])
        zf_list = list(range(NSLOT // 128))
        for nb in range(24):
            w = wa[nb % 2]
            P.dma("sp", lambda e, nb=nb, w=w: e.dma_start(out=w[:, :, :], in_=wada_d[:, :, nb * 512:(nb + 1) * 512]), writes=[w])
            pp = pm[nb % 2]

            def mm(e, w=w, pp=pp):
                for kc in range(KC):
                    ins = e.matmul(pp[0:NB, :], lhsT=cact[:, kc, :], rhs=w[:, kc, :], start=(kc == 0), stop=(kc == KC - 1))
                return ins
            P.op("pe", mm, reads=[cact, w], writes=[pp])
            P.op("dve", lambda e, nb=nb, pp=pp: e.tensor_tensor(out=modsb[:, nb * 512:(nb + 1) * 512], in0=pp[0:NB, :],
                                                                 in1=badaB[:, nb * 512:(nb + 1) * 512], op=ALU.add),
                 reads=[pp, badaB], writes=[modsb])
        P.dma("sp", lambda e: e.dma_start(out=mod_d[:, :], in_=modsb[:, :]), reads=[modsb], writes=[B_mod])
        P.flush()

    def bcast_load(q, dst, src_ap, nparts=128, reads=()):
        return P.dma(q, lambda e: e.dma_start(out=dst[:, :], in_=src_ap.partition_broadcast(nparts)), reads=list(reads), writes=[dst])

    def rms_front(es_bufs, xt, m_scale, m_shift, hout):
        junk, ssq, rstd, hn = es_bufs
        P.op("act", lambda e: e.activation(out=junk[:, :], in_=xt[:, :], func=AF.Square, accum_out=ssq[:, 0:1]),
             reads=[xt], writes=[junk, ssq])
        P.op("act", lambda e: e.activation(out=rstd[:, 0:1], in_=ssq[:, 0:1], func=AF.Sqrt, bias=epsT[:, 0:1], scale=1.0 / D),
             reads=[ssq, epsT], writes=[rstd])
        P.op("dve", lambda e: e.reciprocal(out=rstd[:, 0:1], in_=rstd[:, 0:1]), reads=[rstd], writes=[rstd])
        P.op("dve", lambda e: e.scalar_tensor_tensor(out=hn[:, :], in0=xt[:, :], scalar=rstd[:, 0:1], in1=m_scale[:, :],
                                                     op0=ALU.mult, op1=ALU.mult), reads=[xt, rstd, m_scale], writes=[hn])
        P.op("pool", lambda e: e.tensor_tensor(out=hout[:, :], in0=hn[:, :], in1=m_shift[:, :], op=ALU.add),
             reads=[hn, m_shift], writes=[hout])

    def transposes(src, pT, dstT, s, evac_engs):
        for h in range(2):
            pb = pT[h]

            def tr(e, h=h, pb=pb):
                for k in range(8):
                    kc = h * 8 + k
                    ins = e.transpose(out=pb[:, k * 128:(k + 1) * 128], in_=src[:, kc * 128:(kc + 1) * 128], identity=ident[:, :])
                return ins
            P.op("pe", tr, reads=[src, ident], writes=[pb])
            en = evac_engs[h]
            o_ap = dstT[:, h * 8:(h + 1) * 8, s * 128:(s + 1) * 128]
            i_ap = pb[:, :].rearrange("p (k t) -> p k t", k=8)
            if en == "act":
                P.op("act", lambda e, o_ap=o_ap, i_ap=i_ap: e.copy(out=o_ap, in_=i_ap), reads=[pb], writes=[dstT])
            else:
                P.op(en, lambda e, o_ap=o_ap, i_ap=i_ap: e.tensor_copy(out=o_ap, in_=i_ap), reads=[pb], writes=[dstT])

    if phases < 1:
        P.flush(final=True)
        top.close()
        return nc

    P.new_phase_sems()
    with ExitStack() as es:
        P.barrier()
        m1B = mk(es, "m1B", [128, D], F32)
        shmB = mk(es, "shmB", [128, D], F32)
        gmixB = mk(es, "gmixB", [128, D], F32)
        xs = [mk(es, f"xs{i}", [128, D], F32) for i in range(2)]
        junk = mk(es, "junk", [128, D], BF16)
        htm = [mk(es, f"htm{i}", [128, D], BF16) for i in range(2)]
        ssq = [mk(es, f"ssq{i}", [128, 1], F32) for i in range(2)]
        rstd = [mk(es, f"rstd{i}", [128, 1], F32) for i in range(2)]
        hT = mk(es, "hT", [128, KC, 512], BF16)
        win = [mk(es, f"win{i}", [128, KC, 640], BF16) for i in range(2)]
        yT = mk(es, "yT", [128, KC, 512], BF16)
        uhist = mk(es, "uhist", [128, 8, 2], F32)
        ghist = mk(es, "ghist", [128, 8, 30], F32)
        uw = [mk(es, f"uw{i}", [128, 514], F32) for i in range(2)]
        gw = [mk(es, f"gw{i}", [128, 542], F32) for i in range(2)]
        zb = [mk(es, f"zb{j}", [128, 512], F32) for j in range(8)]
        xa_s = [mk(es, f"xa_s{i}", [128, 512], F32) for i in range(2)]
        acc_a = [mk(es, f"acc_a{i}", [128, 512], F32) for i in range(2)]
        ya = [mk(es, f"ya{i}", [128, 512], F32) for i in range(2)]
        sq = [mk(es, f"sq{i}", [128, 512], F32) for i in range(2)]
        rs = [mk(es, f"rs{i}", [128, 512], F32) for i in range(2)]
        sg = [mk(es, f"sg{i}", [128, 512], F32) for i in range(2)]
        mean = mk(es, "mean", [128, 512], F32)
        msq = mk(es, "msq", [128, 512], F32)
        lrstd = mk(es, "lrstd", [128, 512], F32)
        t1 = [mk(es, f"t1{i}", [128, 512], F32) for i in range(2)]
        zz = [mk(es, f"zz{i}", [128, 512], F32) for i in range(2)]
        onesf = mk(es, "onesf", [128, 128], F32)
        blkf = mk(es, "blkf", [128, 128], F32)
        pT = [mkp(es, f"pT{i}", [128, 1024], BF16) for i in range(2)]
        pr = [mkp(es, f"pr{i}", [128, 512], F32) for i in range(3)]
        pS1 = mkp(es, "pS1", [128, 512], F32)
        pS2 = mkp(es, "pS2", [128, 512], F32)
        ph = mkp(es, "ph", [128, 512], F32)
        prr = [0]

        def next_pr():
            b = pr[prr[0] % 3]
            prr[0] += 1
            return b

        P.op("dve", lambda e: e.memset(onesf[:, :], 1.0), writes=[onesf])
        P.op("pool", lambda e: e.memset(blkf[:, :], 0.0), writes=[blkf])
        P.op("pool", lambda e: e.memset(blkf[0:64, 0:64], 1.0), reads=[blkf], writes=[blkf])
        P.op("pool", lambda e: e.memset(blkf[64:128, 64:128], 1.0), reads=[blkf], writes=[blkf])
        bcast_load("sp", gmixB, rvec_d[0:1, R_GMIX:R_GMIX + D])

        def head_norm(src, gcol, dst_ap, dst_buf, i2):
            P.op("act", lambda e: e.activation(out=sq[i2][:, :], in_=src[:, :], func=AF.Square), reads=[src], writes=[sq[i2]])
            P.op("pe", lambda e: e.matmul(ph[:, :], lhsT=blkf[:, :], rhs=sq[i2][:, :], start=True, stop=True),
                 reads=[blkf, sq[i2]], writes=[ph])
            P.op("act", lambda e: e.activation(out=rs[i2][:, :], in_=ph[:, :], func=AF.Sqrt, bias=epsT[:, 0:1], scale=1.0 / 64),
                 reads=[ph, epsT], writes=[rs[i2]])
            P.op("dve", lambda e: e.reciprocal(out=rs[i2][:, :], in_=rs[i2][:, :]), reads=[rs[i2]], writes=[rs[i2]])
            P.op("dve", lambda e: e.scalar_tensor_tensor(out=dst_ap, in0=src[:, :], scalar=fvec[:, gcol:gcol + 1], in1=rs[i2][:, :],
                                                         op0=ALU.mult, op1=ALU.mult), reads=[src, fvec, rs[i2]], writes=[dst_buf])


        sqa = [mk(es, f"sqa{i}", [128, 512], F32) for i in range(2)]
        ba_s = [mk(es, f"ba_s{i}", [128, 512], F32) for i in range(2)]
        jcount = [0]

        def front(tt):
            b, ti = divmod(tt, T5B)
            if ti == 0:
                bcast_load("sp", m1B, mod_d[b:b + 1, D:2 * D], reads=[B_mod])
                P.op("dve", lambda e: e.scalar_tensor_tensor(out=m1B[:, :], in0=m1B[:, :], scalar=1.0, in1=gmixB[:, :],
                                                             op0=ALU.add, op1=ALU.mult), reads=[m1B, gmixB], writes=[m1B])
                bcast_load("sp", shmB, mod_d[b:b + 1, 0:D], reads=[B_mod])
                P.op("dve", lambda e: e.memset(uhist[:, :, :], 0.0), reads=[uhist], writes=[uhist])
                P.op("dve", lambda e: e.memset(ghist[:, :, :], 0.0), reads=[ghist], writes=[ghist])
            for s in range(4):
                i2 = s % 2
                xt = xs[i2]
                r0 = tt * 512 + s * 128
                P.dma("sp", lambda e, xt=xt, r0=r0: e.dma_start(out=xt[:, :], in_=x_d[r0:r0 + 128, :]), writes=[xt])
                rms_front((junk, ssq[i2], rstd[i2], xt), xt, m1B, shmB, htm[i2])
                transposes(htm[i2], pT, hT, s, ("act", "dve"))

        def hn_tail(src, gcol, dst_ap, dst_buf, i2):
            P.op("act", lambda e: e.activation(out=rs[i2][:, :], in_=ph[:, :], func=AF.Sqrt, bias=epsT[:, 0:1], scale=1.0 / 64),
                 reads=[ph, epsT], writes=[rs[i2]])
            P.op("dve", lambda e: e.reciprocal(out=rs[i2][:, :], in_=rs[i2][:, :]), reads=[rs[i2]], writes=[rs[i2]])
            P.op("dve", lambda e: e.scalar_tensor_tensor(out=dst_ap, in0=src[:, :], scalar=fvec[:, gcol:gcol + 1], in1=rs[i2][:, :],
                                                         op0=ALU.mult, op1=ALU.mult), reads=[src, fvec, rs[i2]], writes=[dst_buf])

        front(0)
        for tt in range(NT5):
            pend = None
            for j in range(8):
                wj = win[jcount[0] % 2]
                jcount[0] += 1
                P.dma("pool", lambda e, wj=wj, j=j: e.dma_start(out=wj[:, :, :], in_=win_d[j]), writes=[wj])
                nzf = -(-(NSLOT // 128) // (NT5 * 8))
                for _ in range(nzf):
                    if zf_list:
                        a = zf_list.pop()
                        P.dma("pool", lambda e, a=a: e.dma_start(out=xb_d[a * 128:(a + 1) * 128, :], in_=zt[:, :]), reads=[zt], writes=[B_xb])

                def proj(c, wj=wj):
                    pb = next_pr()

                    def mm(e, pb=pb, c=c, wj=wj):
                        for kc in range(KC):
                            ins = e.matmul(pb[:, :], lhsT=wj[:, kc, c * 128:(c + 1) * 128], rhs=hT[:, kc, :],
                                           start=(kc == 0), stop=(kc == KC - 1))
                        return ins
                    P.op("pe", mm, reads=[wj, hT], writes=[pb])
                    return pb
                i2 = j % 2
                p_xa = proj(0)
                P.op("act", lambda e, p=p_xa, i2=i2: e.copy(out=xa_s[i2][:, :], in_=p[:, :]), reads=[p_xa], writes=[xa_s[i2]])
                p_ba = proj(1)
                P.op("act", lambda e, p=p_ba, i2=i2: e.copy(out=ba_s[i2][:, :], in_=p[:, :]), reads=[p_ba], writes=[ba_s[i2]])
                p_ca = proj(2)
                u = uw[i2]
                P.op("dve", lambda e, u=u, j=j: e.tensor_copy(out=u[:, 0:2], in_=uhist[:, j, :]), reads=[uhist], writes=[u])
                P.op("dve", lambda e, u=u, p=p_ca, i2=i2: e.tensor_tensor(out=u[:, 2:514], in0=p[:, :], in1=xa_s[i2][:, :], op=ALU.mult),
                     reads=[p_ca, xa_s[i2]], writes=[u])
                p_vb = proj(3)
                p_gb = proj(4)
                P.op("act", lambda e, p=p_gb, i2=i2: e.activation(out=sg[i2][:, :], in_=p[:, :], func=AF.Sigmoid), reads=[p_gb], writes=[sg[i2]])
                g = gw[i2]
                P.op("dve", lambda e, g=g, j=j: e.tensor_copy(out=g[:, 0:30], in_=ghist[:, j, :]), reads=[ghist], writes=[g])
                P.op("dve", lambda e, g=g, p=p_vb, i2=i2: e.tensor_tensor(out=g[:, 30:542], in0=p[:, :], in1=sg[i2][:, :], op=ALU.mult),
                     reads=[p_vb, sg[i2]], writes=[g])
                if pend is not None:
                    pend[0]()
                P.op("dve", lambda e, u=u, j=j: e.tensor_copy(out=uhist[:, j, :], in_=u[:, 512:514]), reads=[u], writes=[uhist])
                aa = acc_a[i2]
                P.op("dve", lambda e, u=u, aa=aa, j=j: e.tensor_scalar(out=aa[:, :], in0=u[:, 0:512], scalar1=fvec[:, FA_W + j * 3:FA_W + j * 3 + 1],
                                                                       scalar2=None, op0=ALU.mult), reads=[u, fvec], writes=[aa])
                for k in (1, 2):
                    P.op("dve", lambda e, u=u, aa=aa, j=j, k=k: e.scalar_tensor_tensor(
                        out=aa[:, :], in0=u[:, k:k + 512], scalar=fvec[:, FA_W + j * 3 + k:FA_W + j * 3 + k + 1], in1=aa[:, :],
                        op0=ALU.mult, op1=ALU.add), reads=[u, fvec, aa], writes=[aa])
                P.op("dve", lambda e, aa=aa, i2=i2: e.tensor_tensor(out=ya[i2][:, :], in0=ba_s[i2][:, :], in1=aa[:, :], op=ALU.mult),
                     reads=[ba_s[i2], aa], writes=[ya[i2]])
                P.op("act", lambda e, i2=i2: e.activation(out=sqa[i2][:, :], in_=ya[i2][:, :], func=AF.Square), reads=[ya[i2]], writes=[sqa[i2]])
                if pend is not None:
                    pend[1]()
                P.op("dve", lambda e, g=g, j=j: e.tensor_copy(out=ghist[:, j, :], in_=g[:, 512:542]), reads=[g], writes=[ghist])
                z = zb[j]
                P.op("dve", lambda e, g=g, z=z, j=j: e.tensor_scalar(out=z[:, :], in0=g[:, 0:512], scalar1=fvec[:, FB_W + j * 31:FB_W + j * 31 + 1],
                                                                     scalar2=fvec[:, FB_B + j:FB_B + j + 1], op0=ALU.mult, op1=ALU.add),
                     reads=[g, fvec], writes=[z])
                for k in range(1, 31):
                    P.op("dve", lambda e, g=g, z=z, j=j, k=k: e.scalar_tensor_tensor(
                        out=z[:, :], in0=g[:, k:k + 512], scalar=fvec[:, FB_W + j * 31 + k:FB_W + j * 31 + k + 1], in1=z[:, :],
                        op0=ALU.mult, op1=ALU.add), reads=[g, fvec, z], writes=[z])
                P.op("act", lambda e, z=z, i2=i2: e.activation(out=sq[i2][:, :], in_=z[:, :], func=AF.Square), reads=[z], writes=[sq[i2]])

                def st2_pe(j=j, i2=i2, z=z):
                    P.op("pe", lambda e: e.matmul(ph[:, :], lhsT=blkf[:, :], rhs=sqa[i2][:, :], start=True, stop=True),
                         reads=[blkf, sqa[i2]], writes=[ph])
                    P.op("pe", lambda e: e.matmul(pS1[:, :], lhsT=onesf[:, :], rhs=z[:, :], start=(j == 0), stop=(j == 7)),
                         reads=[onesf, z], writes=[pS1])
                    P.op("pe", lambda e: e.matmul(pS2[:, :], lhsT=onesf[:, :], rhs=sq[i2][:, :], start=(j == 0), stop=(j == 7)),
                         reads=[onesf, sq[i2]], writes=[pS2])

                def st2_tail(j=j, i2=i2):
                    hn_tail(ya[i2], FHA + j, yT[:, j, :], yT, i2)
                pend = (st2_pe, st2_tail)
            pend[0]()
            pend[1]()
            if tt + 1 < NT5:
                front(tt + 1)
            P.op("act", lambda e: e.activation(out=mean[:, :], in_=pS1[:, :], func=AF.Identity, scale=1.0 / 1024), reads=[pS1], writes=[mean])
            P.op("dve", lambda e: e.tensor_tensor(out=msq[:, :], in0=mean[:, :], in1=mean[:, :], op=ALU.mult), reads=[mean], writes=[msq])
            P.op("dve", lambda e: e.scalar_tensor_tensor(out=msq[:, :], in0=pS2[:, :], scalar=1.0 / 1024, in1=msq[:, :],
                                                         op0=ALU.mult, op1=ALU.subtract), reads=[pS2, msq], writes=[msq])
            P.op("act", lambda e: e.activation(out=lrstd[:, :], in_=msq[:, :], func=AF.Sqrt, bias=epsT[:, 0:1], scale=1.0),
                 reads=[msq, epsT], writes=[lrstd])
            P.op("dve", lambda e: e.reciprocal(out=lrstd[:, :], in_=lrstd[:, :]), reads=[lrstd], writes=[lrstd])
            prev = None
            for j in range(8):
                i2 = j % 2
                z = zb[j]
                P.op("dve", lambda e, z=z, i2=i2: e.tensor_tensor(out=t1[i2][:, :], in0=z[:, :], in1=mean[:, :], op=ALU.subtract),
                     reads=[z, mean], writes=[t1[i2]])
                P.op("pool", lambda e, i2=i2: e.tensor_tensor(out=t1[i2][:, :], in0=t1[i2][:, :], in1=lrstd[:, :], op=ALU.mult),
                     reads=[t1[i2], lrstd], writes=[t1[i2]])
                P.op("act", lambda e, i2=i2, j=j: e.activation(out=zz[i2][:, :], in_=t1[i2][:, :], func=AF.Silu,
                                                               scale=fvec[:, FLN_G + j:FLN_G + j + 1], bias=fvec[:, FLN_B + j:FLN_B + j + 1]),
                     reads=[t1[i2], fvec], writes=[zz[i2]])
                P.op("act", lambda e, i2=i2: e.activation(out=sqa[i2][:, :], in_=zz[i2][:, :], func=AF.Square), reads=[zz[i2]], writes=[sqa[i2]])
                if prev is not None:
                    prev()
                P.op("pe", lambda e, i2=i2: e.matmul(ph[:, :], lhsT=blkf[:, :], rhs=sqa[i2][:, :], start=True, stop=True),
                     reads=[blkf, sqa[i2]], writes=[ph])

                def tail(j=j, i2=i2):
                    hn_tail(zz[i2], FHB + j, yT[:, 8 + j, :], yT, i2)
                prev = tail
            prev()
            P.dma("sp", lambda e, tt=tt: e.dma_start(out=yT_d[tt], in_=yT[:, :, :]), reads=[yT], writes=[B_yT])

        P.flush()

    if phases < 2:
        P.barrier()
        P.flush(final=True)
        top.close()
        return nc

    P.new_phase_sems()
    with ExitStack() as es:
        P.barrier()
        wo = mk(es, "wo", [128, KC, D], BF16)
        gmB = mk(es, "gmB", [128, D], F32)
        yTs = [mk(es, f"yTs{i}", [128, KC, 512], BF16) for i in range(2)]
        xr = [mk(es, f"xr{i}", [128, D], F32) for i in range(3)]
        tmpo = [mk(es, f"tmpo{i}", [128, 512], F32) for i in range(2)]
        po = [mkp(es, f"po{i}", [128, 512], F32) for i in range(4)]
        for q in range(4):
            P.dma("pool", lambda e, q=q: e.dma_start(out=wo[:, q * 4:(q + 1) * 4, :], in_=wout_d[:, q * 4:(q + 1) * 4, :]), writes=[wo])
        cnt = 0
        for tt in range(NT5):
            b, ti = divmod(tt, T5B)
            if ti == 0:
                bcast_load("sp", gmB, mod_d[b:b + 1, 2 * D:3 * D], reads=[B_mod])
            yt = yTs[tt % 2]
            P.dma("sp", lambda e, yt=yt, tt=tt: e.dma_start(out=yt[:, :, :], in_=yT_d[tt]), reads=[B_yT], writes=[yt])
            for s in range(4):
                xt = xr[(tt * 4 + s) % 3]
                r0 = tt * 512 + s * 128
                P.dma("sp", lambda e, xt=xt, r0=r0: e.dma_start(out=xt[:, :], in_=x_d[r0:r0 + 128, :]), writes=[xt])
                for n in range(4):
                    pb = po[cnt % 4]
                    tm = tmpo[cnt % 2]
                    cnt += 1

                    def mm(e, pb=pb, yt=yt, s=s, n=n):
                        for kc in range(KC):
                            ins = e.matmul(pb[:, :], lhsT=yt[:, kc, s * 128:(s + 1) * 128], rhs=wo[:, kc, n * 512:(n + 1) * 512],
                                           start=(kc == 0), stop=(kc == KC - 1))
                        return ins
                    P.op("pe", mm, reads=[yt, wo], writes=[pb])
                    P.op("dve", lambda e, pb=pb, tm=tm, n=n: e.tensor_tensor(out=tm[:, :], in0=pb[:, :], in1=gmB[:, n * 512:(n + 1) * 512], op=ALU.mult),
                         reads=[pb, gmB], writes=[tm])
                    P.op("pool", lambda e, tm=tm, xt=xt, n=n: e.tensor_tensor(out=xt[:, n * 512:(n + 1) * 512], in0=tm[:, :],
                                                                               in1=xt[:, n * 512:(n + 1) * 512], op=ALU.add),
                         reads=[tm, xt], writes=[xt])
                P.dma("sp", lambda e, xt=xt, r0=r0: e.dma_start(out=x1_d[r0:r0 + 128, :], in_=xt[:, :]), reads=[xt], writes=[B_x1])
        P.flush()

    if phases < 3:
        P.barrier()
        P.flush(final=True)
        top.close()
        return nc

    P.new_phase_sems()
    with ExitStack() as es:
        P.barrier()
        m2B = mk(es, "m2B", [128, D], F32)
        shfB = mk(es, "shfB", [128, D], F32)
        gfB = mk(es, "gfB", [128, D], F32)
        x1s = [mk(es, f"x1s{i}", [128, D], F32) for i in range(4)]
        hn = mk(es, "hn2", [128, D], F32)
        junk = mk(es, "junk2", [128, D], BF16)
        h2tm = [mk(es, f"h2tm{i}", [128, D], BF16) for i in range(5)]
        ssq = [mk(es, f"ssq2{i}", [128, 1], F32) for i in range(2)]
        rstd = [mk(es, f"rstd2{i}", [128, 1], F32) for i in range(2)]
        h2T = mk(es, "h2T", [128, KC, 512], BF16)
        wsg = mk(es, "wsg", [128, KC, FE], BF16)
        wsu = mk(es, "wsu", [128, KC, FE], BF16)
        wsd = mk(es, "wsd", [128, 4, D], BF16)
        wrb = mk(es, "wrb", [128, KC, NE], BF16)
        AT = mk(es, "AT", [128, 4, 512], BF16)
        sgs = [mk(es, f"sgs{i}", [128, 512], F32) for i in range(2)]
        tmpo = [mk(es, f"tmp2o{i}", [128, 512], F32) for i in range(2)]
        selall = mk(es, "selall", [128, NT, NE], BF16)
        onesb = mk(es, "onesb", [128, 128], BF16)
        trib = mk(es, "trib", [128, 128], BF16)
        trif = mk(es, "trif", [128, 128], F32)
        rbB = mk(es, "rbB", [128, NE], F32)
        eC1 = mk(es, "eC1", [128, NE], F32)
        eCi = mk(es, "eCi", [128, NE], I32)
        R = {n: mk(es, "r_" + n, [128, NE], F32) for n in ("sc", "bi", "eq", "t", "mk", "masked", "sel", "ws", "lt", "g2", "selv", "pf", "jk")}
        R8 = {n: mk(es, "r8_" + n, [128, 8], F32) for n in ("m1", "m2", "gs", "g8", "gm", "off", "m8", "p8")}
        ssum = mk(es, "ssum", [128, 1], F32)
        pT = [mkp(es, f"pT2{i}", [128, 1024], BF16) for i in range(2)]
        pgu = [mkp(es, f"pgu{i}", [128, 512], F32) for i in range(4)]
        plog = mkp(es, "plog", [128, 512], F32)
        prk = mkp(es, "prk", [128, 512], F32)

        P.dma("pool", lambda e: e.dma_start(out=wsg[:, :, :], in_=wsg_d[:, :, :]), writes=[wsg])
        P.dma("pool", lambda e: e.dma_start(out=wsu[:, :, :], in_=wsu_d[:, :, :]), writes=[wsu])
        P.dma("pool", lambda e: e.dma_start(out=wsd[:, :, :], in_=wsd_d[:, :, :]), writes=[wsd])
        P.dma("pool", lambda e: e.dma_start(out=wrb[:, :, :], in_=wr_d[:, :, :]), writes=[wrb])
        bcast_load("sp", rbB, rvec_d[0:1, R_RB:R_RB + NE])
        P.op("dve", lambda e: e.memset(onesb[:, :], 1.0), writes=[onesb])
        trii = mk(es, "trii", [128, 128], I32)
        P.op("pool", lambda e: e.iota(trii[:, :], pattern=[[1, 128]], base=0, channel_multiplier=-1), writes=[trii])
        P.op("dve", lambda e: e.tensor_copy(out=trif[:, :], in_=trii[:, :]), reads=[trii], writes=[trif])
        P.op("dve", lambda e: e.tensor_scalar(out=trib[:, :], in0=trif[:, :], scalar1=0.0, scalar2=None, op0=ALU.is_gt), reads=[trif], writes=[trib])
        P.op("pool", lambda e: e.iota(eCi[:, :], pattern=[[C, NE]], base=1, channel_multiplier=0), writes=[eCi])
        P.op("dve", lambda e: e.tensor_copy(out=eC1[:, :], in_=eCi[:, :]), reads=[eCi], writes=[eC1])

        def v3(buf):
            return buf[:, :].rearrange("p (g e) -> p g e", g=8)

        def b3(buf8):
            return buf8[:, :].unsqueeze(2).to_broadcast([128, 8, 8])

        cnt = 0
        for tt in range(NT5):
            b, ti = divmod(tt, T5B)
            if ti == 0:
                bcast_load("sp", m2B, mod_d[b:b + 1, 4 * D:5 * D], reads=[B_mod])
                bcast_load("sp", hn, rvec_d[0:1, R_GFFN:R_GFFN + D])
                P.op("dve", lambda e: e.scalar_tensor_tensor(out=m2B[:, :], in0=m2B[:, :], scalar=1.0, in1=hn[:, :],
                                                             op0=ALU.add, op1=ALU.mult), reads=[m2B, hn], writes=[m2B])
                bcast_load("sp", shfB, mod_d[b:b + 1, 3 * D:4 * D], reads=[B_mod])
                bcast_load("sp", gfB, mod_d[b:b + 1, 5 * D:6 * D], reads=[B_mod])
            for s in range(4):
                tile = tt * 4 + s
                i2 = s % 2
                xt = x1s[s]
                r0 = tile * 128
                P.dma("sp", lambda e, xt=xt, r0=r0: e.dma_start(out=xt[:, :], in_=x1_d[r0:r0 + 128, :]), reads=[B_x1], writes=[xt])
                h2 = h2tm[tile % 5]
                rms_front((junk, ssq[i2], rstd[i2], hn), xt, m2B, shfB, h2)
                transposes(h2, pT, h2T, s, ("act", "dve"))

                def mmr(e, s=s):
                    for kc in range(KC):
                        ins = e.matmul(plog[:, 0:NE], lhsT=h2T[:, kc, s * 128:(s + 1) * 128], rhs=wrb[:, kc, :],
                                       start=(kc == 0), stop=(kc == KC - 1))
                    return ins
                P.op("pe", mmr, reads=[h2T, wrb], writes=[plog])
                sc, bi, eq, t_, mkb, masked, sel, ws, lt, g2, selv, pf, jk = (R[n] for n in ("sc", "bi", "eq", "t", "mk", "masked", "sel", "ws", "lt", "g2", "selv", "pf", "jk"))
                m1, m2, gs, g8, gm, off, m8, p8 = (R8[n] for n in ("m1", "m2", "gs", "g8", "gm", "off", "m8", "p8"))
                P.op("act", lambda e: e.activation(out=sc[:, :], in_=plog[:, 0:NE], func=AF.Sigmoid), reads=[plog], writes=[sc])
                P.op("dve", lambda e: e.tensor_tensor(out=bi[:, :], in0=sc[:, :], in1=rbB[:, :], op=ALU.add), reads=[sc, rbB], writes=[bi])
                P.op("dve", lambda e: e.tensor_reduce(out=m1[:, :], in_=v3(bi), axis=AX.X, op=ALU.max), reads=[bi], writes=[m1])
                P.op("dve", lambda e: e.tensor_tensor(out=v3(eq), in0=v3(bi), in1=b3(m1), op=ALU.is_equal), reads=[bi, m1], writes=[eq])
                P.op("dve", lambda e: e.scalar_tensor_tensor(out=t_[:, :], in0=eq[:, :], scalar=-4.0, in1=bi[:, :], op0=ALU.mult, op1=ALU.add),
                     reads=[eq, bi], writes=[t_])
                P.op("dve", lambda e: e.tensor_reduce(out=m2[:, :], in_=v3(t_), axis=AX.X, op=ALU.max), reads=[t_], writes=[m2])
                P.op("dve", lambda e: e.tensor_tensor(out=gs[:, :], in0=m1[:, :], in1=m2[:, :], op=ALU.add), reads=[m1, m2], writes=[gs])
                P.op("dve", lambda e: e.max(out=g8[:, :], in_=gs[:, :]), reads=[gs], writes=[g8])
                P.op("dve", lambda e: e.tensor_scalar(out=gm[:, :], in0=gs[:, :], scalar1=g8[:, 3:4], scalar2=None, op0=ALU.is_ge),
                     reads=[gs, g8], writes=[gm])
                P.op("dve", lambda e: e.tensor_tensor(out=v3(mkb), in0=v3(bi), in1=b3(gm), op=ALU.mult), reads=[bi, gm], writes=[mkb])
                P.op("dve", lambda e: e.tensor_scalar(out=off[:, :], in0=gm[:, :], scalar1=4.0, scalar2=-4.0, op0=ALU.mult, op1=ALU.add),
                     reads=[gm], writes=[off])
                P.op("dve", lambda e: e.tensor_tensor(out=v3(masked), in0=v3(mkb), in1=b3(off), op=ALU.add), reads=[mkb, off], writes=[masked])
                P.op("dve", lambda e: e.max(out=m8[:, :], in_=masked[:, :]), reads=[masked], writes=[m8])
                P.op("dve", lambda e: e.tensor_scalar(out=sel[:, :], in0=masked[:, :], scalar1=m8[:, 7:8], scalar2=None, op0=ALU.is_ge),
                     reads=[masked, m8], writes=[sel])
                P.op("dve", lambda e, tile=tile: e.tensor_copy(out=selall[:, tile, :], in_=sel[:, :]), reads=[sel], writes=[selall])
                P.op("dve", lambda e: e.memset(ssum[:, :], 0.0), reads=[ssum], writes=[ssum])
                P.op("dve", lambda e: e.scalar_tensor_tensor(out=ws[:, :], in0=sc[:, :], scalar=1.0, in1=sel[:, :], op0=ALU.mult, op1=ALU.mult,
                                                             accum_out=ssum[:, 0:1]), reads=[sc, sel, ssum], writes=[ws, ssum])
                P.op("dve", lambda e: e.reciprocal(out=ssum[:, :], in_=ssum[:, :]), reads=[ssum], writes=[ssum])
                P.op("dve", lambda e: e.tensor_scalar(out=ssum[:, :], in0=ssum[:, :], scalar1=2.5, scalar2=None, op0=ALU.mult),
                     reads=[ssum], writes=[ssum])

                def mrk(e, tile=tile):
                    for jt in range(tile):
                        e.matmul(prk[:, 0:NE], lhsT=onesb[:, :], rhs=selall[:, jt, :], start=(jt == 0), stop=False)
                    return e.matmul(prk[:, 0:NE], lhsT=trib[:, :], rhs=selall[:, tile, :], start=(tile == 0), stop=True)
                P.op("pe", mrk, reads=[onesb, trib, selall], writes=[prk])
                P.op("dve", lambda e: e.tensor_scalar(out=lt[:, :], in0=prk[:, 0:NE], scalar1=float(C), scalar2=None, op0=ALU.is_lt),
                     reads=[prk], writes=[lt])
                P.op("dve", lambda e: e.scalar_tensor_tensor(out=g2[:, :], in0=ws[:, :], scalar=ssum[:, 0:1], in1=lt[:, :], op0=ALU.mult, op1=ALU.mult),
                     reads=[ws, ssum, lt], writes=[g2])
                P.op("dve", lambda e: e.tensor_tensor(out=selv[:, :], in0=sel[:, :], in1=lt[:, :], op=ALU.mult), reads=[sel, lt], writes=[selv])
                P.op("dve", lambda e: e.tensor_tensor(out=pf[:, :], in0=prk[:, 0:NE], in1=eC1[:, :], op=ALU.add), reads=[prk, eC1], writes=[pf])
                P.op("dve", lambda e: e.tensor_tensor(out=pf[:, :], in0=pf[:, :], in1=selv[:, :], op=ALU.mult), reads=[pf, selv], writes=[pf])
                P.op("dve", lambda e: e.tensor_scalar(out=pf[:, :], in0=pf[:, :], scalar1=-1.0, scalar2=None, op0=ALU.add), reads=[pf], writes=[pf])
                P.op("dve", lambda e: e.max(out=p8[:, :], in_=pf[:, :]), reads=[pf], writes=[p8])
                P.op("dve", lambda e: e.tensor_scalar(out=R8["m2"][:, :], in0=p8[:, :], scalar1=0.0, scalar2=None, op0=ALU.is_lt), reads=[p8], writes=[R8["m2"]])
                P.op("dve", lambda e: e.scalar_tensor_tensor(out=R8["m1"][:, :], in0=R8["m2"][:, :], scalar=600000.0, in1=p8[:, :], op0=ALU.mult, op1=ALU.add),
                     reads=[R8["m2"], p8], writes=[R8["m1"]])
                P.op("dve", lambda e, tile=tile: e.tensor_copy(out=posall[:, tile * 8:(tile + 1) * 8], in_=R8["m1"][:, :]), reads=[R8["m1"]], writes=[posall])
                for k in range(8):
                    P.op("dve", lambda e, tile=tile, k=k: e.scalar_tensor_tensor(
                        out=jk[:, :], in0=pf[:, :], scalar=p8[:, k:k + 1], in1=g2[:, :], op0=ALU.is_equal, op1=ALU.mult,
                        accum_out=wall[:, tile * 8 + k:tile * 8 + k + 1]), reads=[pf, p8, g2, wall], writes=[jk, wall])
                for k in range(8):
                    P.dma("pool", lambda e, h2=h2, tile=tile, k=k: e.indirect_dma_start(
                        out=xb_d[:, :], out_offset=bass.IndirectOffsetOnAxis(ap=posall[:, tile * 8 + k:tile * 8 + k + 1], axis=0),
                        in_=h2[:, :], in_offset=None, bounds_check=get_bc(e, 2), oob_is_err=False),
                        reads=[h2, posall, B_xb], writes=[B_xb])
                if dbg:
                    P.dma("sp", lambda e, r0=r0, tile=tile: e.dma_start(out=dbg_d[r0:r0 + 128, 8:16], in_=wall[:, tile * 8:(tile + 1) * 8]),
                          reads=[wall], writes=[B_out])
                    P.dma("sp", lambda e, r0=r0: e.dma_start(out=dbg_d[r0:r0 + 128, 0:8], in_=R8["p8"][:, :]),
                          reads=[R8["p8"]], writes=[B_out])
            for fc in range(4):
                pg = pgu[(2 * fc) % 4]
                pu = pgu[(2 * fc + 1) % 4]

                def mmg(e, pg=pg, fc=fc, w=wsg):
                    for kc in range(KC):
                        ins = e.matmul(pg[:, :], lhsT=w[:, kc, fc * 128:(fc + 1) * 128], rhs=h2T[:, kc, :], start=(kc == 0), stop=(kc == KC - 1))
                    return ins

                def mmu(e, pu=pu, fc=fc, w=wsu):
                    for kc in range(KC):
                        ins = e.matmul(pu[:, :], lhsT=w[:, kc, fc * 128:(fc + 1) * 128], rhs=h2T[:, kc, :], start=(kc == 0), stop=(kc == KC - 1))
                    return ins
                P.op("pe", mmg, reads=[wsg, h2T], writes=[pg])
                P.op("pe", mmu, reads=[wsu, h2T], writes=[pu])
                sgb = sgs[fc % 2]
                P.op("act", lambda e, pg=pg, sgb=sgb: e.activation(out=sgb[:, :], in_=pg[:, :], func=AF.Silu), reads=[pg], writes=[sgb])
                P.op("dve", lambda e, pu=pu, sgb=sgb, fc=fc: e.tensor_tensor(out=AT[:, fc, :], in0=pu[:, :], in1=sgb[:, :], op=ALU.mult),
                     reads=[pu, sgb], writes=[AT])
            for s in range(4):
                xt = x1s[s]
                r0 = (tt * 4 + s) * 128
                for n in range(4):
                    pb = pgu[cnt % 4]
                    tm = tmpo[cnt % 2]
                    cnt += 1

                    def mmd(e, pb=pb, s=s, n=n):
                        for fc in range(4):
                            ins = e.matmul(pb[:, :], lhsT=AT[:, fc, s * 128:(s + 1) * 128], rhs=wsd[:, fc, n * 512:(n + 1) * 512],
                                           start=(fc == 0), stop=(fc == 3))
                        return ins
                    P.op("pe", mmd, reads=[AT, wsd], writes=[pb])
                    P.op("dve", lambda e, pb=pb, tm=tm, n=n: e.tensor_tensor(out=tm[:, :], in0=pb[:, :], in1=gfB[:, n * 512:(n + 1) * 512], op=ALU.mult),
                         reads=[pb, gfB], writes=[tm])
                    P.op("pool", lambda e, tm=tm, xt=xt, n=n: e.tensor_tensor(out=xt[:, n * 512:(n + 1) * 512], in0=tm[:, :],
                                                                               in1=xt[:, n * 512:(n + 1) * 512], op=ALU.add),
                         reads=[tm, xt], writes=[xt])
                P.dma("sp", lambda e, xt=xt, r0=r0: e.dma_start(out=x2_d[r0:r0 + 128, :], in_=xt[:, :]), reads=[xt], writes=[B_x2])

        def mcnt(e):
            for jt in range(NT):
                ins = e.matmul(prk[0:1, 0:NE], lhsT=onesb[:, 0:1], rhs=selall[:, jt, :], start=(jt == 0), stop=(jt == NT - 1))
            return ins
        P.op("pe", mcnt, reads=[onesb, selall], writes=[prk])
        P.cnt_ev = P.op("dve", lambda e: e.tensor_copy(out=cnts[0:1, :], in_=prk[0:1, 0:NE]), reads=[prk], writes=[cnts])
        P.cnt_ap = lambda idx: cnts[0:1, idx:idx + 1]
        P.flush()

    if phases < 4:
        P.barrier()
        P.flush(final=True)
        top.close()
        return nc

    P.new_phase_sems()
    with ExitStack() as es:
        P.barrier()
        wg = [mk(es, f"wg{i}", [128, KC, FE], BF16) for i in range(2)]
        wu = [mk(es, f"wu{i}", [128, KC, FE], BF16) for i in range(2)]
        wd = [mk(es, f"wd{i}", [128, 4, D], BF16) for i in range(2)]
        xrow = [mk(es, f"xrow{i}", [128, D], BF16) for i in range(8)]
        XT = [mk(es, f"XT{i}", [128, KC, 512], BF16) for i in range(2)]
        ATe = [mk(es, f"ATe{i}", [128, 4, 512], BF16) for i in range(2)]
        sge = [mk(es, f"sge{i}", [128, 512], F32) for i in range(2)]
        yst = [mk(es, f"yst{i}", [128, D], BF16) for i in range(4)]
        pT = [mkp(es, f"pT3{i}", [128, 1024], BF16) for i in range(2)]
        pgu = [mkp(es, f"pgu3{i}", [128, 512], F32) for i in range(4)]
        py = [mkp(es, f"py3{i}", [128, 512], F32) for i in range(2)]
        gcnt = 0
        rowc = 0
        ycnt = 0
        yscnt = 0

        wgc = [[wg[i]] for i in range(2)]
        wuc = [[wu[i]] for i in range(2)]
        wdc = [[wd[i]] for i in range(2)]

        def load_w(e_):
            i = e_ % 2
            P.dma("pool", lambda e: e.dma_start(out=wg[i][:, :, :], in_=wg_d[e_]), writes=[wg[i]])
            P.dma("pool", lambda e: e.dma_start(out=wu[i][:, :, :], in_=wu_d[e_]), writes=[wu[i]])
            P.dma("pool", lambda e: e.dma_start(out=wd[i][:, :, :], in_=wd_d[e_]), writes=[wd[i]])

        load_w(0)
        for ex in range(NE):
            if ex + 1 < NE:
                load_w(ex + 1)
            wi = ex % 2
            for (g0, gn) in groups:
                P.begin_cond(ex, g0)
                xt_ = XT[gcnt % 2]
                at_ = ATe[gcnt % 2]
                gcnt += 1
                ns = gn // 128
                for s in range(ns):
                    xr_ = xrow[rowc % 8]
                    rowc += 1
                    r0 = ex * C + g0 + s * 128
                    P.dma("sp", lambda e, xr_=xr_, r0=r0: e.dma_start(out=xr_[:, :], in_=xb_d[r0:r0 + 128, :]), reads=[B_xb], writes=[xr_])
                    transposes(xr_, pT, xt_, s, ("act", "dve"))
                for fc in range(4):
                    pg = pgu[(2 * fc) % 4]
                    pu = pgu[(2 * fc + 1) % 4]

                    def mmg(e, pg=pg, fc=fc, w=wg[wi], xt_=xt_, gn=gn):
                        for kc in range(KC):
                            ins = e.matmul(pg[:, 0:gn], lhsT=w[:, kc, fc * 128:(fc + 1) * 128], rhs=xt_[:, kc, 0:gn], start=(kc == 0), stop=(kc == KC - 1))
                        return ins

                    def mmu(e, pu=pu, fc=fc, w=wu[wi], xt_=xt_, gn=gn):
                        for kc in range(KC):
                            ins = e.matmul(pu[:, 0:gn], lhsT=w[:, kc, fc * 128:(fc + 1) * 128], rhs=xt_[:, kc, 0:gn], start=(kc == 0), stop=(kc == KC - 1))
                        return ins
                    P.op("pe", mmg, reads=wgc[wi] + [xt_], writes=[pg])
                    P.op("pe", mmu, reads=wuc[wi] + [xt_], writes=[pu])
                    sgb = sge[fc % 2]
                    P.op("act", lambda e, pg=pg, sgb=sgb, gn=gn: e.activation(out=sgb[:, 0:gn], in_=pg[:, 0:gn], func=AF.Silu), reads=[pg], writes=[sgb])
                    P.op("dve", lambda e, pu=pu, sgb=sgb, fc=fc, at_=at_, gn=gn: e.tensor_tensor(out=at_[:, fc, 0:gn], in0=pu[:, 0:gn], in1=sgb[:, 0:gn], op=ALU.mult),
                         reads=[pu, sgb], writes=[at_])
                for s in range(ns):
                    ys = yst[yscnt % 4]
                    yscnt += 1
                    r0 = ex * C + g0 + s * 128
                    for n in range(4):
                        pb = py[ycnt % 2]
                        ycnt += 1

                        def mmd(e, pb=pb, s=s, n=n, at_=at_, w=wd[wi]):
                            for fc in range(4):
                                ins = e.matmul(pb[:, :], lhsT=at_[:, fc, s * 128:(s + 1) * 128], rhs=w[:, fc, n * 512:(n + 1) * 512],
                                               start=(fc == 0), stop=(fc == 3))
                            return ins
                        P.op("pe", mmd, reads=[at_] + wdc[wi], writes=[pb])
                        if n % 2 == 0:
                            P.op("act", lambda e, pb=pb, ys=ys, n=n: e.copy(out=ys[:, n * 512:(n + 1) * 512], in_=pb[:, :]), reads=[pb], writes=[ys])
                        else:
                            P.op("dve", lambda e, pb=pb, ys=ys, n=n: e.tensor_copy(out=ys[:, n * 512:(n + 1) * 512], in_=pb[:, :]), reads=[pb], writes=[ys])
                    P.dma("act", lambda e, ys=ys, r0=r0: e.dma_start(out=yb_d[r0:r0 + 128, :], in_=ys[:, :]), reads=[ys], writes=[B_yb])
                P.end_cond()
        P.flush()

    if phases < 5:
        P.barrier()
        P.flush(final=True)
        top.close()
        return nc

    P.new_phase_sems()
    with ExitStack() as es:
        P.barrier()
        NG = 16
        gsl = [mk(es, f"gsl{i}", [128, D], BF16) for i in range(NG)]
        gfB = mk(es, "gfB4", [128, D], F32)
        gfinB = mk(es, "gfinB", [128, D], F32)
        x2s = [mk(es, f"x2s{i}", [128, D], F32) for i in range(2)]
        acc = [mk(es, f"acc4{i}", [128, D], F32) for i in range(2)]
        junk = mk(es, "junk4", [128, D], BF16)
        ssq = [mk(es, f"ssq4{i}", [128, 1], F32) for i in range(2)]
        rstd = [mk(es, f"rstd4{i}", [128, 1], F32) for i in range(2)]
        for i in range(NG):
            P.op("pool" if i % 2 else "dve", lambda e, i=i: e.memset(gsl[i][:, :], 0.0), writes=[gsl[i]])
        bcast_load("sp", gfinB, rvec_d[0:1, R_GFIN:R_GFIN + D])
        gc = 0
        gather_evs = []
        for tile in range(NT):
            b = (tile * 128) // S
            if (tile * 128) % S == 0:
                bcast_load("sp", gfB, mod_d[b:b + 1, 5 * D:6 * D], reads=[B_mod])
            i2 = tile % 2
            r0 = tile * 128
            xt = x2s[i2]
            P.dma("sp", lambda e, xt=xt, r0=r0: e.dma_start(out=xt[:, :], in_=x2_d[r0:r0 + 128, :]), reads=[B_x2], writes=[xt])
            a = acc[i2]
            for k in range(8):
                gt = gsl[gc % NG]
                gc += 1
                if True:
                  P.dma("pool", lambda e, gt=gt, tile=tile, k=k: e.indirect_dma_start(
                    out=gt[:, :], out_offset=None, in_=yb_d[:, :],
                    in_offset=bass.IndirectOffsetOnAxis(ap=posall[:, tile * 8 + k:tile * 8 + k + 1], axis=0),
                    bounds_check=get_bc(e, 4), oob_is_err=False), reads=[B_yb, posall, gt], writes=[gt],
                    extra=[gather_evs[-GSER]] if len(gather_evs) >= GSER else [])
                  gather_evs.append(gt.lw)
                wcol = wall[:, tile * 8 + k:tile * 8 + k + 1]
                if k == 0:
                    P.op("dve", lambda e, gt=gt, a=a, wcol=wcol: e.tensor_scalar(out=a[:, :], in0=gt[:, :], scalar1=wcol, scalar2=None, op0=ALU.mult),
                         reads=[gt, wall], writes=[a])
                else:
                    P.op("dve", lambda e, gt=gt, a=a, wcol=wcol: e.scalar_tensor_tensor(out=a[:, :], in0=gt[:, :], scalar=wcol, in1=a[:, :],
                                                                                         op0=ALU.mult, op1=ALU.add), reads=[gt, wall, a], writes=[a])
            P.op("pool", lambda e, a=a: e.tensor_tensor(out=a[:, :], in0=a[:, :], in1=gfB[:, :], op=ALU.mult), reads=[a, gfB], writes=[a])
            P.op("pool", lambda e, a=a, xt=xt: e.tensor_tensor(out=xt[:, :], in0=a[:, :], in1=xt[:, :], op=ALU.add), reads=[a, xt], writes=[xt])
            P.op("act", lambda e, xt=xt, i2=i2: e.activation(out=junk[:, :], in_=xt[:, :], func=AF.Square, accum_out=ssq[i2][:, 0:1]),
                 reads=[xt], writes=[junk, ssq[i2]])
            P.op("act", lambda e, i2=i2: e.activation(out=rstd[i2][:, 0:1], in_=ssq[i2][:, 0:1], func=AF.Sqrt, bias=epsT[:, 0:1], scale=1.0 / D),
                 reads=[ssq[i2], epsT], writes=[rstd[i2]])
            P.op("dve", lambda e, i2=i2: e.reciprocal(out=rstd[i2][:, 0:1], in_=rstd[i2][:, 0:1]), reads=[rstd[i2]], writes=[rstd[i2]])
            P.op("dve", lambda e, xt=xt, a=a, i2=i2: e.scalar_tensor_tensor(out=a[:, :], in0=xt[:, :], scalar=rstd[i2][:, 0:1], in1=gfinB[:, :],
                                                                            op0=ALU.mult, op1=ALU.mult), reads=[xt, rstd[i2], gfinB], writes=[a])
            P.dma("sp", lambda e, a=a, r0=r0: e.dma_start(out=out_d[r0:r0 + 128, :], in_=a[:, :]), reads=[a], writes=[B_out])
        P.flush(final=True)
    top.close()
    return nc


def prep_shared(inp):
    f32 = np.float32
    sh = {}
    sh["w_ada"] = np.ascontiguousarray(inp["w_ada"][0].reshape(KC, 128, 6 * D).transpose(1, 0, 2))
    W = inp["w_in"][0]
    sh["w_in"] = np.ascontiguousarray(W.reshape(KC, 128, 5, 8, 128).transpose(3, 1, 0, 2, 4).reshape(8, 128, KC, 640))
    sh["w_out"] = np.ascontiguousarray(inp["w_out"][0].reshape(KC, 128, D).transpose(1, 0, 2))
    sh["w_router"] = np.ascontiguousarray(inp["w_router"][0].reshape(KC, 128, NE).transpose(1, 0, 2))
    sh["ws_gate"] = np.ascontiguousarray(inp["w_shared_gate"][0].reshape(KC, 128, FE).transpose(1, 0, 2))
    sh["ws_up"] = np.ascontiguousarray(inp["w_shared_up"][0].reshape(KC, 128, FE).transpose(1, 0, 2))
    sh["ws_down"] = np.ascontiguousarray(inp["w_shared_down"][0].reshape(4, 128, D).transpose(1, 0, 2))
    sh["w_gate"] = np.ascontiguousarray(inp["w_gate"][0].reshape(NE, KC, 128, FE).transpose(0, 2, 1, 3))
    sh["w_up"] = np.ascontiguousarray(inp["w_up"][0].reshape(NE, KC, 128, FE).transpose(0, 2, 1, 3))
    sh["w_down"] = np.ascontiguousarray(inp["w_down"][0].reshape(NE, 4, 128, D).transpose(0, 2, 1, 3))
    fv = np.zeros((128, NF), f32)
    fv[:, FA_W:FA_W + 24] = inp["conv_a_w"][0].reshape(3, 8, 128).transpose(2, 1, 0).reshape(128, 24)
    fv[:, FB_W:FB_W + 248] = inp["conv_b_w"][0].reshape(31, 8, 128).transpose(2, 1, 0).reshape(128, 248)
    for off, key in ((FB_B, "conv_b_b"), (FLN_G, "ln_b_g"), (FLN_B, "ln_b_b"), (FHA, "head_norm_a_g"), (FHB, "head_norm_b_g")):
        fv[:, off:off + 8] = inp[key][0].reshape(8, 128).T
    sh["fvec"] = fv
    rv = np.zeros((1, NR), f32)
    rv[0, R_GMIX:R_GMIX + D] = inp["norm_mix_g"][0]
    rv[0, R_GFFN:R_GFFN + D] = inp["norm_ffn_g"][0]
    rv[0, R_GFIN:R_GFIN + D] = inp["norm_final_g"]
    rv[0, R_RB:R_RB + NE] = inp["router_bias"][0]
    rv[0, R_BADA:R_BADA + 6 * D] = inp["b_ada"][0]
    sh["rvec"] = rv
    return sh


def core_inputs(sh, x_core, c_core):
    NB = c_core.shape[0]
    m = dict(sh)
    m["x"] = np.ascontiguousarray(x_core.reshape(-1, D))
    m["cT"] = np.ascontiguousarray(c_core.reshape(NB, KC, 128).transpose(2, 1, 0))
    return m


CAP = 1024
GSER = 2


def kernel(**inputs):
    inp = {k: np.asarray(v) for k, v in inputs.items()}
    x = inp["x"]
    B, S, _ = x.shape
    NB = B // NCORES
    sh = prep_shared(inp)
    in_maps = [core_inputs(sh, x[i * NB:(i + 1) * NB], inp["c"][i * NB:(i + 1) * NB]) for i in range(NCORES)]
    nc = build(NB, S, CAP)
    res = run_bass_kernel_spmd(nc, in_maps, core_ids=list(range(NCORES)))
    out = np.concatenate([r["out"].reshape(NB, S, D) for r in res.results], axis=0)
    return out.astype(np.float32)
```
